# Optimizing a Trainium2 kernel written in Bass

```python
import jax, jax.numpy as jnp
from jax import lax
import numpy as np

D_MODEL = 1024
BATCH = 8
SEQ = 4096
DEPTH = 2

HEAD_DIM = 64
N_HEADS = D_MODEL // HEAD_DIM
N_MOBA = N_HEADS // 4
N_FOX = (N_HEADS - N_MOBA) // 2
N_DIL = N_HEADS - N_MOBA - N_FOX
N_ALIBI = N_MOBA + N_DIL
MOBA_BLOCK = 256
MOBA_TOPK = 3
MOBA_QCHUNK = 32
FOX_QBLOCK = 128
DIL_PAIRS = ((128, 1), (512, 4), (2048, 16))
DIL_BLOCK = 128
D_FF = 4 * D_MODEL
QKV_COLS = 3 * D_MODEL
FOX_GATE_COLS = N_FOX * HEAD_DIM
N_IN_COLS = QKV_COLS + N_FOX + FOX_GATE_COLS
W_MOBA = N_MOBA * HEAD_DIM
W_FOX = N_FOX * HEAD_DIM
ATTN_SCALE = HEAD_DIM ** -0.5
EPS = 1e-6
NEG_INF = -1e30

kernel_name = "hymba_moba_fox_dilated_block"


def rms_norm(x, gain):
    xf = x.astype(jnp.float32)
    y = xf * lax.rsqrt(jnp.mean(xf * xf, axis=-1, keepdims=True) + EPS)
    return (y * gain.astype(jnp.float32)).astype(x.dtype)


def alibi_slopes():
    return jnp.exp2(-8.0 * jnp.arange(1, N_ALIBI + 1, dtype=jnp.float32) / N_ALIBI)


def moba_attention(q, k, v, slopes):
    b, h, s, dh = q.shape
    n_blk = -(-s // MOBA_BLOCK)
    s_pad = n_blk * MOBA_BLOCK
    pad = ((0, 0), (0, 0), (0, s_pad - s), (0, 0))
    q, k, v = jnp.pad(q, pad), jnp.pad(k, pad), jnp.pad(v, pad)
    k_blocks = k.reshape(b, h, n_blk, MOBA_BLOCK, dh)
    v_blocks = v.reshape(b, h, n_blk, MOBA_BLOCK, dh)
    k_mean = jnp.mean(k_blocks.astype(jnp.float32), axis=3)
    gate = jnp.einsum('bhsd,bhnd->bhsn', q.astype(jnp.float32), k_mean)
    q_blk = jnp.arange(s_pad) // MOBA_BLOCK
    past = jnp.arange(n_blk)[None, :] < q_blk[:, None]
    gate = jnp.where(past, gate, NEG_INF)
    top_k = min(MOBA_TOPK, n_blk)
    _, sel_idx = lax.top_k(gate, top_k)
    sel_ok = sel_idx < q_blk[:, None]
    gather = jax.vmap(jax.vmap(lambda blocks, idx: blocks[idx]))
    offs = jnp.arange(MOBA_BLOCK)

    def chunk(c):
        start = c * MOBA_QCHUNK
        qc = lax.dynamic_slice_in_dim(q, start, MOBA_QCHUNK, axis=2)
        idx = lax.dynamic_slice_in_dim(sel_idx, start, MOBA_QCHUNK, axis=2)
        ok = lax.dynamic_slice_in_dim(sel_ok, start, MOBA_QCHUNK, axis=2)
        q_pos = start + jnp.arange(MOBA_QCHUNK)
        k_sel = gather(k_blocks, idx)
        v_sel = gather(v_blocks, idx)
        d_sel = (q_pos[:, None, None] - (idx[..., None] * MOBA_BLOCK + offs)).astype(jnp.float32)
        l_sel = (jnp.einsum('bhqd,bhqnkd->bhqnk', qc, k_sel).astype(jnp.float32) * ATTN_SCALE
                 - slopes[:, None, None, None] * d_sel)
        l_sel = jnp.where(ok[..., None], l_sel, NEG_INF).reshape(b, h, MOBA_QCHUNK, top_k * MOBA_BLOCK)
        own_start = (start // MOBA_BLOCK) * MOBA_BLOCK
        k_own = lax.dynamic_slice_in_dim(k, own_start, MOBA_BLOCK, axis=2)
        v_own = lax.dynamic_slice_in_dim(v, own_start, MOBA_BLOCK, axis=2)
        d_own = q_pos[:, None] - (own_start + offs)[None, :]
        l_own = (jnp.einsum('bhqd,bhkd->bhqk', qc, k_own).astype(jnp.float32) * ATTN_SCALE
                 - slopes[:, None, None] * d_own.astype(jnp.float32))
        l_own = jnp.where(d_own >= 0, l_own, NEG_INF)
        p = jax.nn.softmax(jnp.concatenate([l_sel, l_own], axis=-1), axis=-1).astype(v.dtype)
        p_sel = p[..., :top_k * MOBA_BLOCK].reshape(b, h, MOBA_QCHUNK, top_k, MOBA_BLOCK)
        return (jnp.einsum('bhqnk,bhqnkd->bhqd', p_sel, v_sel)
                + jnp.einsum('bhqk,bhkd->bhqd', p[..., top_k * MOBA_BLOCK:], v_own))

    out = lax.map(chunk, jnp.arange(s_pad // MOBA_QCHUNK))
    return out.transpose(1, 2, 0, 3, 4).reshape(b, h, s_pad, dh)[:, :, :s]


def forgetting_attention(q, k, v, f_logit):
    b, h, s, dh = q.shape
    cum = jnp.cumsum(jax.nn.log_sigmoid(f_logit.astype(jnp.float32)), axis=1).transpose(0, 2, 1)
    k_pos = jnp.arange(s)

    def block(i):
        start = i * FOX_QBLOCK
        qb = lax.dynamic_slice_in_dim(q, start, FOX_QBLOCK, axis=2)
        cb = lax.dynamic_slice_in_dim(cum, start, FOX_QBLOCK, axis=2)
        q_pos = start + jnp.arange(FOX_QBLOCK)
        logits = (jnp.einsum('bhqd,bhkd->bhqk', qb, k).astype(jnp.float32) * ATTN_SCALE
                  + cb[..., None] - cum[:, :, None, :])
        logits = jnp.where(k_pos[None, :] <= q_pos[:, None], logits, NEG_INF)
        p = jax.nn.softmax(logits, axis=-1).astype(v.dtype)
        return jnp.einsum('bhqk,bhkd->bhqd', p, v)

    out = lax.map(block, jnp.arange(s // FOX_QBLOCK))
    return out.transpose(1, 2, 0, 3, 4).reshape(b, h, s, dh)


def dilated_branch(q, k, v, slopes, window, dilation):
    b, h, s, dh = q.shape
    steps = window // dilation
    sub_len = s // dilation
    n_blk = -(-sub_len // DIL_BLOCK)
    sub_pad = n_blk * DIL_BLOCK

    def to_sub(t):
        t = t.reshape(b, h, sub_len, dilation, dh).transpose(0, 1, 3, 2, 4)
        t = jnp.pad(t, ((0, 0), (0, 0), (0, 0), (0, sub_pad - sub_len), (0, 0)))
        return t.reshape(b, h, dilation, n_blk, DIL_BLOCK, dh)

    def band(t):
        prev = jnp.concatenate([jnp.zeros_like(t[:, :, :, :1]), t[:, :, :, :-1]], axis=3)
        return jnp.concatenate([prev, t], axis=4)

    qs = to_sub(q)
    kb, vb = band(to_sub(k)), band(to_sub(v))
    blk = jnp.arange(n_blk)[:, None]
    i_q = blk * DIL_BLOCK + jnp.arange(DIL_BLOCK)[None, :]
    i_k = (blk - 1) * DIL_BLOCK + jnp.arange(2 * DIL_BLOCK)[None, :]
    off = i_q[:, :, None] - i_k[:, None, :]
    allowed = (off >= 0) & (off <= steps) & (i_k[:, None, :] >= 0) & (i_k[:, None, :] < sub_len)
    dist = (off * dilation).astype(jnp.float32)
    logits = (jnp.einsum('bhrnqd,bhrnkd->bhrnqk', qs, kb).astype(jnp.float32) * ATTN_SCALE
              - slopes[:, None, None, None, None] * dist)
    logits = jnp.where(allowed, logits, NEG_INF)
    lse = jax.nn.logsumexp(logits, axis=-1)
    p = jnp.exp(logits - lse[..., None]).astype(v.dtype)
    o = jnp.einsum('bhrnqk,bhrnkd->bhrnqd', p, vb)
    o = o.reshape(b, h, dilation, sub_pad, dh)[:, :, :, :sub_len].transpose(0, 1, 3, 2, 4).reshape(b, h, s, dh)
    lse = lse.reshape(b, h, dilation, sub_pad)[..., :sub_len].transpose(0, 1, 3, 2).reshape(b, h, s)
    return o, lse


def dilated_attention(q, k, v, slopes):
    outs, lses = [], []
    for window, dilation in DIL_PAIRS:
        o, lse = dilated_branch(q, k, v, slopes, window, dilation)
        outs.append(o)
        lses.append(lse)
    w = jax.nn.softmax(jnp.stack(lses, axis=0), axis=0)
    o = jnp.sum(w[..., None] * jnp.stack(outs, axis=0).astype(jnp.float32), axis=0)
    return o.astype(q.dtype)


def to_bsd(o):
    b, h, s, dh = o.shape
    return o.transpose(0, 2, 1, 3).reshape(b, s, h * dh)


def mixer_sublayer(x, norm_g, w_in, b_f, q_gain, k_gain, out_gain, w_out):
    b, s, _ = x.shape
    hn = rms_norm(x, norm_g)
    proj = hn @ w_in
    q = proj[..., :D_MODEL].reshape(b, s, N_HEADS, HEAD_DIM)
    k = proj[..., D_MODEL:2 * D_MODEL].reshape(b, s, N_HEADS, HEAD_DIM)
    v = proj[..., 2 * D_MODEL:QKV_COLS].reshape(b, s, N_HEADS, HEAD_DIM)
    f_logit = proj[..., QKV_COLS:QKV_COLS + N_FOX] + b_f
    g_fox = proj[..., QKV_COLS + N_FOX:]
    q = rms_norm(q, q_gain).transpose(0, 2, 1, 3)
    k = rms_norm(k, k_gain).transpose(0, 2, 1, 3)
    v = v.transpose(0, 2, 1, 3)
    slopes = alibi_slopes()
    a0, a1 = N_MOBA, N_MOBA + N_FOX
    o_moba = moba_attention(q[:, :a0], k[:, :a0], v[:, :a0], slopes[N_DIL:])
    o_fox = forgetting_attention(q[:, a0:a1], k[:, a0:a1], v[:, a0:a1], f_logit)
    o_dil = dilated_attention(q[:, a1:], k[:, a1:], v[:, a1:], slopes[:N_DIL])
    y_moba = rms_norm(to_bsd(o_moba), out_gain[:W_MOBA])
    y_fox = rms_norm(to_bsd(o_fox), out_gain[W_MOBA:W_MOBA + W_FOX]) * jax.nn.sigmoid(g_fox)
    y_dil = rms_norm(to_bsd(o_dil), out_gain[W_MOBA + W_FOX:])
    y = jnp.concatenate([y_moba, y_fox, y_dil], axis=-1) @ w_out
    return x + y


def mlp_sublayer(x, norm_g, w_up, w_down):
    hn = rms_norm(x, norm_g)
    return x + jnp.square(jax.nn.relu(hn @ w_up)) @ w_down


def setup_inputs(seed: int = 0) -> dict:
    key = jax.random.key(seed)
    ks = jax.random.split(key, 11)
    f32 = jnp.float32
    return {
        "x": jax.random.normal(ks[0], (BATCH, SEQ, D_MODEL), f32),
        "attn_norm": 1.0 + 0.02 * jax.random.normal(ks[1], (DEPTH, D_MODEL), f32),
        "w_in": jax.random.normal(ks[2], (DEPTH, D_MODEL, N_IN_COLS), f32) * D_MODEL ** -0.5,
        "b_forget": 2.0 + 0.5 * jax.random.normal(ks[3], (DEPTH, N_FOX), f32),
        "q_gain": 1.0 + 0.02 * jax.random.normal(ks[4], (DEPTH, N_HEADS, HEAD_DIM), f32),
        "k_gain": 1.0 + 0.02 * jax.random.normal(ks[5], (DEPTH, N_HEADS, HEAD_DIM), f32),
        "out_gain": 1.0 + 0.02 * jax.random.normal(ks[6], (DEPTH, D_MODEL), f32),
        "w_out": jax.random.normal(ks[7], (DEPTH, D_MODEL, D_MODEL), f32) * D_MODEL ** -0.5,
        "mlp_norm": 1.0 + 0.02 * jax.random.normal(ks[8], (DEPTH, D_MODEL), f32),
        "w_up": jax.random.normal(ks[9], (DEPTH, D_MODEL, D_FF), f32) * D_MODEL ** -0.5,
        "w_down": jax.random.normal(ks[10], (DEPTH, D_FF, D_MODEL), f32) * D_FF ** -0.5,
    }


def reference(x, attn_norm, w_in, b_forget, q_gain, k_gain, out_gain, w_out, mlp_norm, w_up, w_down):
    for l in range(DEPTH):
        x = mixer_sublayer(x, attn_norm[l], w_in[l], b_forget[l], q_gain[l], k_gain[l],
                           out_gain[l], w_out[l])
        x = mlp_sublayer(x, mlp_norm[l], w_up[l], w_down[l])
    return x
```

```python
from contextlib import ExitStack
import numpy as np
import ml_dtypes
import concourse.bass as bass
import concourse.mybir as mybir
from concourse.bass_utils import run_bass_kernel_spmd

F32 = mybir.dt.float32
BF16 = mybir.dt.bfloat16
ALU = mybir.AluOpType
AF = mybir.ActivationFunctionType
AX = mybir.AxisListType

PE, ACT, DVE, POOL, SP = "tensor", "scalar", "vector", "gpsimd", "sync"
ENGS = (PE, ACT, DVE, POOL, SP)

D = 1024
NH = 16
DH = 64
NCOL = 3462
DFF = 4096
EPS = 1e-6
N_FOX = 6
MASKNEG = -1024.0


class Op:
    __slots__ = ("eng", "fn", "deps", "signal", "sem", "val", "is_dma")


class Buf:
    def __init__(self, name):
        self.name = name
        self.writers = []
        self.readers = []
        self.ld = None
        self.st = None


def _compress(lst):
    best = {}
    for o in lst:
        key = o.sem if o.is_dma else o.eng
        cur = best.get(key)
        if cur is None or o.val > cur.val:
            best[key] = o
    return list(best.values())


class Sched:
    def __init__(self, nc):
        self.nc = nc
        self.ops = {e: [] for e in ENGS}
        self.seq = 0
        self._semctx = []
        self.pool = {}
        self.live = []
        self.last_compute = {}
        self.dma_latest = {}
        self.engsem = {e: self.new_sem("prog_" + e) for e in ENGS}

    def new_sem(self, name):
        ctx = self.nc.semaphore(name)
        s = ctx.__enter__()
        self._semctx.append(ctx)
        return s

    def close(self):
        for ctx in reversed(self._semctx):
            ctx.__exit__(None, None, None)

    def _mk(self, eng, fn, reads, writes, pwrites):
        o = Op()
        o.eng, o.fn, o.signal, o.sem, o.is_dma = eng, fn, False, None, False
        self.seq += 1
        o.val = self.seq
        deps = []
        for b in reads:
            deps.extend(b.writers)
        for b in writes:
            deps.extend(b.writers)
            deps.extend(b.readers)
        for b in pwrites:
            deps.extend(b.writers)
            deps.extend(b.readers)
        o.deps = deps
        return o

    def _commit(self, o, reads, writes, pwrites):
        o.deps = _compress(o.deps)
        for b in reads:
            b.readers.append(o)
            if len(b.readers) > 12:
                b.readers = _compress(b.readers)
        for b in writes:
            b.writers = [o]
            b.readers = []
        for b in pwrites:
            b.writers.append(o)
            if len(b.writers) > 12:
                b.writers = _compress(b.writers)
        self.ops[o.eng].append(o)
        if o.is_dma:
            self.dma_latest[id(o.sem)] = o
        else:
            self.last_compute[o.eng] = o

    def op(self, eng, fn, reads=(), writes=(), pwrites=()):
        o = self._mk(eng, fn, reads, writes, pwrites)
        self._commit(o, reads, writes, pwrites)
        return o

    def _slot(self, sb, store, eng):
        slot = sb.st if store else sb.ld
        if slot is None:
            pool = self.pool.setdefault(eng, [])
            slot = pool.pop() if pool else [self.new_sem("dma%d" % len(self._semctx)), 0, None, eng]
            if store:
                sb.st = slot
            else:
                sb.ld = slot
            self.live.append(sb)
        return slot

    def dma(self, eng, fn, sb, store=False, reads=(), writes=(), pwrites=()):
        o = self._mk(eng, fn, reads, writes, pwrites)
        o.is_dma = True
        slot = self._slot(sb, store, eng)
        slot[1] += 16
        o.sem, o.val = slot[0], slot[1]
        if len(slot) > 2 and slot[2] is not None:
            o.deps.append(slot[2])
        if len(slot) > 2:
            slot[2] = o
        else:
            slot.append(o)
        self._commit(o, reads, writes, pwrites)
        return o

    def barrier(self):
        deps = list(self.last_compute.values()) + list(self.dma_latest.values())
        for e in ENGS:
            o = Op()
            o.eng, o.fn, o.signal, o.sem, o.is_dma = e, (lambda eng: None), False, None, False
            self.seq += 1
            o.val = self.seq
            o.deps = list(deps)
            self.ops[e].append(o)
        for b in self.live:
            if b.ld is not None:
                self.pool.setdefault(b.ld[3], []).append(b.ld)
                b.ld = None
            if b.st is not None:
                self.pool.setdefault(b.st[3], []).append(b.st)
                b.st = None
        self.live = []

    def emit(self, final_waits=()):
        nc = self.nc
        for e in ENGS:
            for o in self.ops[e]:
                for d in o.deps:
                    if not d.is_dma and (d.eng != o.eng or o.is_dma or d.eng != PE):
                        d.signal = True
        for e in ENGS:
            c = 0
            for o in self.ops[e]:
                if not o.is_dma and o.signal:
                    c += 1
                    o.val = c
                    o.sem = self.engsem[e]
        ops = self.ops

        def run(eng_name, e):
            waited = {}
            for o in ops[eng_name]:
                need = {}
                for d in o.deps:
                    if not d.is_dma and d.eng == o.eng and not o.is_dma and d.eng == PE:
                        continue
                    k = id(d.sem)
                    if k not in need or need[k][1] < d.val:
                        need[k] = (d.sem, d.val)
                for k, (sem, v) in need.items():
                    if waited.get(k, 0) < v:
                        e.wait_ge(sem, v)
                        waited[k] = v
                ins = o.fn(e)
                if ins is None:
                    continue
                if o.is_dma:
                    ins.then_inc(o.sem, 16)
                elif o.signal:
                    ins.then_inc(o.sem, 1)
            if eng_name == SP:
                for o in final_waits:
                    e.wait_ge(o.sem, o.val)

        with nc.allow_low_precision(reason="bf16 matmul operands are produced by fp32 math"), nc.Block() as block:
            @block.sync
            def _(e):
                run(SP, e)

            @block.tensor
            def _(e):
                run(PE, e)

            @block.scalar
            def _(e):
                run(ACT, e)

            @block.vector
            def _(e):
                run(DVE, e)

            @block.gpsimd
            def _(e):
                run(POOL, e)


def _split3(a):
    a = a.astype(np.float32)
    h = a.astype(ml_dtypes.bfloat16)
    r1 = a - h.astype(np.float32)
    m = r1.astype(ml_dtypes.bfloat16)
    r2 = r1 - m.astype(np.float32)
    lo = r2.astype(ml_dtypes.bfloat16)
    return np.stack([h, m, lo], axis=-2)


def make_consts(S):
    bf = ml_dtypes.bfloat16
    NT = S // 128
    c = {}
    c["ident"] = np.eye(128, dtype=np.float32).astype(bf)
    j = np.arange(128)[:, None]
    t = np.arange(128)[None, :]
    c["tri"] = np.where(j <= t, 0.0, -30000.0).astype(np.float32)
    band = np.zeros((128, 256), np.float32)
    band[:, 0:128] = np.where(j >= t, 0.0, -30000.0)
    band[:, 128:256] = np.where(j <= t, 0.0, -30000.0)
    c["band"] = np.concatenate([band, band], axis=1).astype(np.float32)
    onesf = np.zeros((65, 64), np.float32)
    onesf[64, :] = 1.0
    c["onesf"] = onesf
    blk = np.zeros((128, 128), np.float32)
    blk[0:64, 0:64] = 1.0 / 64
    blk[64:128, 64:128] = 1.0 / 64
    c["blk"] = blk.astype(bf)
    c["c65"] = np.ones((64, 1), np.float32).astype(bf)
    slopes = np.exp2(-8.0 * np.arange(1, 11, dtype=np.float32) / 10).astype(np.float32)
    hs = np.zeros(16, np.float32)
    hs[0:4] = slopes[6:10]
    hs[10:16] = slopes[0:6]
    pos = (hs[:, None] * np.arange(S, dtype=np.float32)[None, :]).astype(np.float32)
    c["posB"] = _split3(pos)
    c["posA"] = (-c["posB"].astype(np.float32)).astype(bf)
    c["ones3"] = np.ones((3, S), np.float32).astype(bf)
    n = np.arange(16)[:, None]
    c["ind16"] = ((np.arange(S)[None, :] // 256) == n).astype(np.float32).astype(bf)
    qb = (np.arange(NT) // 2)[:, None]
    nn = np.arange(16)[None, :]
    negm = np.where(nn < qb, 0.0, -1e30).astype(np.float32).reshape(1, NT * 16)
    past = (nn < qb).astype(np.float32).reshape(1, NT * 16)
    own = (nn == qb).astype(np.float32).reshape(1, NT * 16)
    c["negm"] = np.ascontiguousarray(np.broadcast_to(negm, (128, NT * 16)))
    c["past"] = np.ascontiguousarray(np.broadcast_to(past, (128, NT * 16)))
    c["own"] = np.ascontiguousarray(np.broadcast_to(own, (128, NT * 16)))
    return c


CONST_DT = {"ident": BF16, "tri": F32, "band": F32, "blk": BF16, "c65": BF16, "onesf": F32, "posA": BF16,
            "posB": BF16, "ones3": BF16, "ind16": BF16, "negm": F32, "past": F32, "own": F32}


def build(S=4096, NL=2, debug=False):
    NT = S // 128
    NCH = S // 512
    nc = bass.Bass("TRN2", target_bir_lowering=False)
    consts_np = make_consts(S)

    def din(name, shape, dt=F32):
        return nc.dram_tensor(name, list(shape), dt, kind="ExternalInput").ap()

    x_in = din("x", [S, D])
    attn_norm = din("attn_norm", [NL, D])
    w_in = din("w_in", [NL, D, NCOL])
    negb_in = din("b_forget", [NL, N_FOX, 1])
    qg_col = din("qg_col", [NL, 128, 8])
    kg_col = din("kg_col", [NL, 128, 8])
    og_col = din("og_col", [NL, 64, 16])
    w_out = din("w_out", [NL, D, D])
    mlp_norm = din("mlp_norm", [NL, D])
    w_up = din("w_up", [NL, D, DFF])
    w_down = din("w_down", [NL, DFF, D])
    cd = {k: din("c_" + k, v.shape, CONST_DT[k]) for k, v in consts_np.items()}
    out = nc.dram_tensor("out", [S, D], F32, kind="ExternalOutput").ap()
    skind = "ExternalOutput" if debug else "Internal"

    def dscr(name, shape, dt):
        return nc.dram_tensor(name, list(shape), dt, kind=skind).ap()

    QT = dscr("QT", [D, S], BF16)
    KT = dscr("KT", [D, S], BF16)
    V = dscr("V", [S, NH, 65], BF16)
    GT = dscr("GT", [N_FOX * 64, S], BF16)
    CUMA = dscr("CUMA", [N_FOX, 3, S], BF16)
    CUMB = dscr("CUMB", [N_FOX, 3, S], BF16)
    YT = dscr("YT", [D, S], BF16)
    X1 = dscr("X1", [S, D], F32)
    XM = dscr("XM", [S, D], F32) if debug else None
    DBGQ = dscr("DBGQ", [86, S], BF16) if debug else None
    bDBG = Buf("DBG")

    S_ = Sched(nc)
    bQT, bKT, bV, bGT, bCA, bCB, bYT, bX1, bOUT = [Buf(n) for n in
                                                   ("QT", "KT", "V", "GT", "CA", "CB", "YT", "X1", "OUT")]
    bXM = Buf("XM")

    with ExitStack() as top:
        uid = [0]

        def sbt(es, name, shape, dt):
            uid[0] += 1
            return es.enter_context(nc.sbuf_tensor("%s_u%d" % (name, uid[0]), list(shape), dt))

        pbank = [top.enter_context(nc.psum_tensor("pb%d" % i, [128, 512], F32)) for i in range(8)]
        bP = [Buf("pb%d" % i) for i in range(8)]

        ident = sbt(top, "ident", [128, 128], BF16)
        tri = sbt(top, "tri", [128, 128], F32)
        band = sbt(top, "band", [128, 512], F32)
        blk = sbt(top, "blk", [128, 128], BF16)
        c65 = sbt(top, "c65", [64, 1], BF16)
        onesf = sbt(top, "onesf", [65, 64], F32)
        SSQ = sbt(top, "SSQ", [128, NT, 3], F32)
        RG = sbt(top, "RG", [128, NT, 3], F32)
        bSSQ = Buf("SSQ")
        bRG = Buf("RG")
        epsb = sbt(top, "epsb", [128, 1], F32)
        oneb = sbt(top, "oneb", [128, 1], F32)
        qsb = sbt(top, "qsb", [128, 1], F32)
        zerob = sbt(top, "zerob", [128, 1], F32)
        bC = Buf("consts")
        for nm, t_ in (("ident", ident), ("tri", tri), ("band", band), ("blk", blk), ("c65", c65), ("onesf", onesf)):
            S_.dma(SP, (lambda e, t_=t_, nm=nm: e.dma_start(out=t_[:], in_=cd[nm])), bC, pwrites=[bC])
        S_.op(POOL, lambda e: e.memset(epsb[:], EPS), pwrites=[bC])
        S_.op(POOL, lambda e: e.memset(oneb[:], 1.0), pwrites=[bC])
        S_.op(POOL, lambda e: e.memset(qsb[:], float(-np.log(8.0))), pwrites=[bC])
        S_.op(POOL, lambda e: e.memset(zerob[:], 0.0), pwrites=[bC])

        def phase1(l, x_src, bx_src):
            with ExitStack() as es:
                w_sb = sbt(es, "w_in_sb", [128, 8, NCOL], BF16)
                bWs = [Buf("w_in%d" % i) for i in range(4)]
                for i in range(4):
                    S_.dma(POOL, (lambda e, i=i: e.dma_start(
                        out=w_sb[:, 2 * i:2 * i + 2, :],
                        in_=w_in[l, 256 * i:256 * (i + 1), :].rearrange("(k p) n -> p k n", p=128))),
                           bWs[i], writes=[bWs[i]])
                g1 = sbt(es, "g1", [128, D], F32)
                gq = sbt(es, "gq", [128, 16], F32)
                negb = sbt(es, "negb", [N_FOX, 1], F32)
                ones6 = sbt(es, "ones6", [N_FOX, 512], F32)
                bG = Buf("p1consts")
                S_.dma(SP, lambda e: e.dma_start(out=g1[:], in_=attn_norm[l:l + 1, :].partition_broadcast(128)),
                       bG, pwrites=[bG])
                S_.dma(SP, lambda e: e.dma_start(out=gq[:, 0:8], in_=qg_col[l]), bG, pwrites=[bG])
                S_.dma(SP, lambda e: e.dma_start(out=gq[:, 8:16], in_=kg_col[l]), bG, pwrites=[bG])
                S_.dma(SP, lambda e: e.dma_start(out=negb[:], in_=negb_in[l]), bG, pwrites=[bG])
                S_.op(DVE, lambda e: e.tensor_scalar(out=negb[:], in0=negb[:], scalar1=-1.0, scalar2=None,
                                                     op0=ALU.mult), reads=[bG], pwrites=[bG])
                S_.op(POOL, lambda e: e.memset(ones6[:], 1.0), pwrites=[bG])

                xt = [sbt(es, "xt%d" % i, [128, D], F32) for i in range(2)]
                bxt = [Buf("xt%d" % i) for i in range(2)]
                st4 = [sbt(es, "st4_%d" % i, [128, 4], F32) for i in range(2)]
                bst = [Buf("st4_%d" % i) for i in range(2)]
                xs = [sbt(es, "xs%d" % i, [128, D], BF16) for i in range(4)]
                bxs = [Buf("xs%d" % i) for i in range(4)]
                hnT = [sbt(es, "hnT%d" % i, [128, 8, 512], BF16) for i in range(2)]
                bhn = [Buf("hnT%d" % i) for i in range(2)]
                sq = [sbt(es, "sq%d" % i, [128, 512], BF16) for i in range(2)]
                bsq = [Buf("sq%d" % i) for i in range(2)]
                lnt = [sbt(es, "lnt%d" % i, [128, 512], F32) for i in range(2)]
                blnt = [Buf("lnt%d" % i) for i in range(2)]
                qo = [sbt(es, "qo%d" % i, [128, 512], BF16) for i in range(3)]
                bqo = [Buf("qo%d" % i) for i in range(3)]
                vst = [sbt(es, "vst%d" % i, [128, NH, 65], BF16) for i in range(2)]
                bvst = [Buf("vst%d" % i) for i in range(2)]
                for i in range(2):
                    S_.op(POOL, (lambda e, i=i: e.memset(vst[i][:, :, 64:65], 1.0)), pwrites=[bvst[i]])
                cn = [sbt(es, "cn%d" % i, [N_FOX, 512], F32) for i in range(2)]
                bcn = [Buf("cn%d" % i) for i in range(2)]
                fr = sbt(es, "fr", [N_FOX, 512], F32)
                bfr = Buf("fr")
                fr2 = sbt(es, "fr2", [N_FOX, 512], F32)
                bfr2 = Buf("fr2")
                pa3 = [sbt(es, "pa3_%d" % i, [N_FOX, 3, 512], BF16) for i in range(2)]
                pb3 = [sbt(es, "pb3_%d" % i, [N_FOX, 3, 512], BF16) for i in range(2)]
                bp3 = [Buf("p3_%d" % i) for i in range(2)]

                def stageA1(c):
                    for tt in range(4):
                        k = (c * 4 + tt) % 2
                        k4 = tt
                        t0 = c * 512 + tt * 128
                        S_.dma(SP, (lambda e, k=k, t0=t0: e.dma_start(out=xt[k][:], in_=x_src[t0:t0 + 128, :])),
                               bxt[k], reads=[bx_src], writes=[bxt[k]])
                        S_.op(ACT, (lambda e, k=k, k4=k4: e.activation(out=xs[k4][:], in_=xt[k][:], func=AF.Square)),
                              reads=[bxt[k]], writes=[bxs[k4]])
                        S_.op(DVE, (lambda e, k=k, k4=k4: e.tensor_reduce(out=st4[k][:, 0:1], in_=xs[k4][:], axis=AX.X,
                                                                          op=ALU.add)),
                              reads=[bxs[k4]], writes=[bst[k]])
                        S_.op(ACT, (lambda e, k=k: e.activation(out=st4[k][:, 1:2], in_=st4[k][:, 0:1], func=AF.Ln,
                                                                bias=epsb[:], scale=1.0 / D)),
                              reads=[bC], writes=[bst[k]])
                        S_.op(ACT, (lambda e, k=k: e.activation(out=st4[k][:, 2:3], in_=st4[k][:, 1:2], func=AF.Exp,
                                                                scale=-0.5)), writes=[bst[k]])
                        S_.op(DVE, (lambda e, k=k, k4=k4: e.scalar_tensor_tensor(out=xs[k4][:], in0=xt[k][:],
                                                                                 scalar=st4[k][:, 2:3], in1=g1[:],
                                                                                 op0=ALU.mult, op1=ALU.mult)),
                              reads=[bxt[k], bst[k], bG], writes=[bxs[k4]])

                def stageA2(c, tt):
                    hb = c % 2
                    k4 = tt
                    pbf = pbank[0][:].bitcast(BF16)

                    def tr(e):
                        ins = None
                        for dc in range(8):
                            ins = e.transpose(pbf[:, dc * 128:(dc + 1) * 128], xs[k4][:, dc * 128:(dc + 1) * 128],
                                              ident[:])
                        return ins
                    S_.op(PE, tr, reads=[bxs[k4], bC], writes=[bP[0]])
                    S_.op(DVE, lambda e: e.tensor_copy(out=hnT[hb][:, :, tt * 128:(tt + 1) * 128],
                                                       in_=pbf.rearrange("p (c t) -> p c t", c=8)),
                          reads=[bP[0]], pwrites=[bhn[hb]])

                def proj(hb, col0, ncols, pidx, tcols=slice(0, 512)):
                    def f(e):
                        ins = None
                        for dc in range(8):
                            ins = e.matmul(pbank[pidx][0:ncols, :], lhsT=w_sb[:, dc, col0:col0 + ncols],
                                           rhs=hnT[hb][:, dc, :], start=(dc == 0), stop=(dc == 7))
                        return ins
                    S_.op(PE, f, reads=bWs + [bhn[hb]], writes=[bP[pidx]])

                cnt = {"qo": 0, "sq": 0}

                def stageB(c):
                    hb = c % 2
                    cs = slice(c * 512, (c + 1) * 512)
                    if c + 1 < NCH:
                        stageA1(c + 1)
                    PA = (1, 2, 5)
                    proj(hb, 0, 128, PA[0])
                    for j in range(16):
                        pa = PA[j % 3]
                        pm = 3 + (j % 2)
                        if c + 1 < NCH and j % 4 == 2:
                            stageA2(c + 1, j // 4)
                        if j + 1 < 16:
                            proj(hb, (j + 1) * 128, 128, PA[(j + 1) % 3])
                        si = cnt["sq"] % 2
                        cnt["sq"] += 1
                        S_.op(ACT, (lambda e, pa=pa, si=si: e.activation(out=sq[si][:], in_=pbank[pa][:],
                                                                         func=AF.Square)),
                              reads=[bP[pa]], writes=[bsq[si]])
                        S_.op(PE, (lambda e, pm=pm, si=si: e.matmul(pbank[pm][:], lhsT=blk[:], rhs=sq[si][:],
                                                                    start=True, stop=True)),
                              reads=[bsq[si], bC], writes=[bP[pm]])
                        S_.op(ACT, (lambda e, pm=pm, si=si: e.activation(out=lnt[si][:], in_=pbank[pm][:],
                                                                         func=AF.Ln, bias=epsb[:])),
                              reads=[bP[pm], bC], writes=[blnt[si]])
                        bias_t = qsb if j < 8 else zerob
                        S_.op(ACT, (lambda e, si=si, bias_t=bias_t: e.activation(out=lnt[si][:], in_=lnt[si][:],
                                                                                 func=AF.Exp, scale=-0.5,
                                                                                 bias=bias_t[:])),
                              reads=[bC], writes=[blnt[si]])
                        qi = cnt["qo"] % 3
                        cnt["qo"] += 1
                        S_.op(DVE, (lambda e, pa=pa, si=si, qi=qi, j=j: e.scalar_tensor_tensor(
                            out=qo[qi][:], in0=pbank[pa][:], scalar=gq[:, j:j + 1], in1=lnt[si][:],
                            op0=ALU.mult, op1=ALU.mult)),
                              reads=[bP[pa], blnt[si], bG], writes=[bqo[qi]])
                        dst, bdst = (QT, bQT) if j < 8 else (KT, bKT)
                        r0 = (j % 8) * 128
                        S_.dma(SP, (lambda e, qi=qi, dst=dst, r0=r0: e.dma_start(out=dst[r0:r0 + 128, cs],
                                                                                 in_=qo[qi][:])),
                               bqo[qi], store=True, reads=[bqo[qi]], pwrites=[bdst])
                    for i in range(3):
                        pa = 1 + (i % 2)
                        proj(hb, 3078 + i * 128, 128, pa)
                        si = cnt["sq"] % 2
                        cnt["sq"] += 1
                        S_.op(ACT, (lambda e, pa=pa, si=si: e.activation(out=lnt[si][:], in_=pbank[pa][:],
                                                                         func=AF.Exp, scale=-1.0)),
                              reads=[bP[pa]], writes=[blnt[si]])
                        S_.op(DVE, (lambda e, si=si: e.tensor_scalar(out=lnt[si][:], in0=lnt[si][:], scalar1=1.0,
                                                                     scalar2=None, op0=ALU.add)),
                              writes=[blnt[si]])
                        qi = cnt["qo"] % 3
                        cnt["qo"] += 1
                        S_.op(DVE, (lambda e, si=si, qi=qi: e.reciprocal(out=qo[qi][:], in_=lnt[si][:])),
                              reads=[blnt[si]], writes=[bqo[qi]])
                        S_.dma(SP, (lambda e, qi=qi, i=i: e.dma_start(out=GT[i * 128:(i + 1) * 128, cs],
                                                                      in_=qo[qi][:])),
                               bqo[qi], store=True, reads=[bqo[qi]], pwrites=[bGT])
                    proj(hb, 3072, N_FOX, 7)
                    S_.op(ACT, lambda e: e.activation(out=fr[:], in_=pbank[7][0:N_FOX, :], func=AF.Exp,
                                                      scale=-1.0, bias=negb[:]),
                          reads=[bP[7], bG], writes=[bfr])
                    S_.op(ACT, lambda e: e.activation(out=fr[:], in_=fr[:], func=AF.Ln, bias=oneb[0:N_FOX, :]),
                          reads=[bC], writes=[bfr])
                    ci = c % 2
                    init = 0.0 if c == 0 else cn[1 - ci][:, 511:512]
                    S_.op(DVE, (lambda e, ci=ci, init=init: e.tensor_tensor_scan(
                        out=cn[ci][:], data0=ones6[:], data1=fr[:], initial=init, op0=ALU.mult, op1=ALU.add)),
                          reads=[bfr, bG, bcn[1 - ci]], writes=[bcn[ci]])
                    S_.op(DVE, (lambda e, ci=ci: e.tensor_copy(out=pb3[ci][:, 0, :], in_=cn[ci][:])),
                          reads=[bcn[ci]], writes=[bp3[ci]])
                    S_.op(DVE, (lambda e, ci=ci: e.tensor_tensor(out=fr[:], in0=cn[ci][:], in1=pb3[ci][:, 0, :],
                                                                 op=ALU.subtract)),
                          reads=[bcn[ci], bp3[ci]], writes=[bfr])
                    S_.op(DVE, (lambda e, ci=ci: e.tensor_copy(out=pb3[ci][:, 1, :], in_=fr[:])),
                          reads=[bfr], pwrites=[bp3[ci]])
                    S_.op(DVE, (lambda e, ci=ci: e.tensor_tensor(out=fr2[:], in0=fr[:], in1=pb3[ci][:, 1, :],
                                                                 op=ALU.subtract)),
                          reads=[bfr, bp3[ci]], writes=[bfr2])
                    S_.op(DVE, (lambda e, ci=ci: e.tensor_copy(out=pb3[ci][:, 2, :], in_=fr2[:])),
                          reads=[bfr2], pwrites=[bp3[ci]])
                    S_.op(DVE, (lambda e, ci=ci: e.tensor_scalar(out=pa3[ci][:], in0=pb3[ci][:], scalar1=-1.0,
                                                                 scalar2=None, op0=ALU.mult)),
                          reads=[bp3[ci]], pwrites=[bp3[ci]])
                    S_.dma(SP, (lambda e, ci=ci: e.dma_start(out=CUMA[:, :, cs], in_=pa3[ci][:])),
                           bp3[ci], store=True, reads=[bp3[ci]], pwrites=[bCA])
                    S_.dma(SP, (lambda e, ci=ci: e.dma_start(out=CUMB[:, :, cs], in_=pb3[ci][:])),
                           bp3[ci], store=True, reads=[bp3[ci]], pwrites=[bCB])
                    for tt in range(4):
                        vi = (c * 4 + tt) % 2
                        t0 = c * 512 + tt * 128
                        for half in range(2):
                            pv = 5 + half

                            def f(e, pv=pv, tt=tt, half=half):
                                ins = None
                                for dc in range(8):
                                    ins = e.matmul(pbank[pv][:], lhsT=hnT[hb][:, dc, tt * 128:(tt + 1) * 128],
                                                   rhs=w_sb[:, dc, 2048 + half * 512:2048 + (half + 1) * 512],
                                                   start=(dc == 0), stop=(dc == 7))
                                return ins
                            S_.op(PE, f, reads=bWs + [bhn[hb]], writes=[bP[pv]])
                            S_.op(ACT, (lambda e, pv=pv, vi=vi, half=half: e.copy(
                                out=vst[vi][:, half * 8:(half + 1) * 8, 0:64],
                                in_=pbank[pv][:].rearrange("p (h d) -> p h d", h=8))),
                                  reads=[bP[pv]], pwrites=[bvst[vi]])
                        S_.dma(SP, (lambda e, vi=vi, t0=t0: e.dma_start(out=V[t0:t0 + 128, :, :], in_=vst[vi][:])),
                               bvst[vi], store=True, reads=[bvst[vi]], pwrites=[bV])

                stageA1(0)
                for tt in range(4):
                    stageA2(0, tt)
                for c in range(NCH):
                    stageB(c)
            S_.barrier()

        def phase2(l):
            with ExitStack() as es:
                ogc = sbt(es, "ogc", [64, 16], F32)
                negm = sbt(es, "negm", [128, NT * 16], F32)
                past = sbt(es, "past", [128, NT * 16], F32)
                own = sbt(es, "own", [128, NT * 16], F32)
                bK = Buf("p2consts")
                S_.dma(SP, lambda e: e.dma_start(out=ogc[:], in_=og_col[l]), bK, pwrites=[bK])
                for nm, t_ in (("negm", negm), ("past", past), ("own", own)):
                    S_.dma(SP, (lambda e, t_=t_, nm=nm: e.dma_start(out=t_[:], in_=cd[nm])), bK, pwrites=[bK])
                RMAX = 86
                qa = [sbt(es, "qa%d" % i, [RMAX, S], BF16) for i in range(2)]
                ka = [sbt(es, "ka%d" % i, [RMAX, S], BF16) for i in range(2)]
                bqa = [Buf("qa%d" % i) for i in range(2)]
                bka = [Buf("ka%d" % i) for i in range(2)]
                va = [sbt(es, "va%d" % i, [128, NT, 65], BF16) for i in range(2)]
                bva = [Buf("va%d" % i) for i in range(2)]
                vd = [[sbt(es, "vd%d_%d" % (j, i), [128, NT, 65], BF16) for i in range(2)] for j in range(2)]
                bvd = [[[Buf("vd%d_%d_%d" % (j, i, r)) for r in range(dd)] for i, dd in enumerate((4, 16))]
                       for j in range(2)]
                pT = [sbt(es, "pT%d" % i, [128, 512], BF16) for i in range(4)]
                bpT = [Buf("pT%d" % i) for i in range(4)]
                sq65 = sbt(es, "sq65", [65, 512], BF16)
                bsq65 = Buf("sq65")
                rden = sbt(es, "rden", [65, 512], F32)
                brden = Buf("rden")
                t0 = sbt(es, "t0", [64, 512], F32)
                bt0 = Buf("t0")
                S_.op(POOL, lambda e: e.memset(SSQ[:], 0.0), writes=[bSSQ])
                lnf = sbt(es, "lnf", [64, 512], F32)
                blnf = Buf("lnf")
                yf = [sbt(es, "yf%d" % i, [64, 512], BF16) for i in range(2)]
                byf = [Buf("yf%d" % i) for i in range(2)]
                gt = [sbt(es, "gt%d" % i, [64, 512], BF16) for i in range(2)]
                bgt = [Buf("gt%d" % i) for i in range(2)]
                kms = sbt(es, "kms", [64, 16], F32)
                kmb = sbt(es, "kmb", [64, 16], BF16)
                bkm = Buf("km")
                gm2 = [sbt(es, "gm%d" % i, [128, 16], F32) for i in range(3)]
                top82 = [sbt(es, "top8%d" % i, [128, 8], F32) for i in range(3)]
                t12 = [sbt(es, "t1%d" % i, [128, 16], F32) for i in range(3)]
                t22 = [sbt(es, "t2%d" % i, [128, 16], F32) for i in range(3)]
                bgm2, btop82, bt12, bt22 = [[Buf("%s%d" % (n, i)) for i in range(3)] for n in ("gm", "top8", "t1", "t2")]
                zp = [sbt(es, "zp%d" % i, [128, 80], BF16) for i in range(3)]
                bzp = [Buf("zp%d" % i) for i in range(3)]
                zero1 = sbt(es, "zero1", [1, 128], BF16)
                for i in range(3):
                    S_.op(POOL, (lambda e, i=i: e.memset(zp[i][:], 0.0)), writes=[bzp[i]])
                S_.op(POOL, lambda e: e.memset(zero1[:], 0.0), pwrites=[bK])
                ycnt = [0]

                def finalize(h, pob, c, gated_gi=None):
                    cs = slice(c * 512, (c + 1) * 512)
                    g = 0 if h < 4 else (1 if h < 10 else 2)
                    def partA():
                        S_.op(ACT, lambda e: e.activation(out=rden[64:65, :], in_=pbank[pob][64:65, :], func=AF.Ln),
                              reads=[bP[pob]], writes=[brden])

                    def partB():
                        S_.op(PE, lambda e: e.matmul(pbank[7][0:64, :], lhsT=onesf[64:65, :], rhs=rden[64:65, :],
                                                     start=True, stop=True),
                              reads=[brden, bC], writes=[bP[7]])
                        S_.op(ACT, lambda e: e.activation(out=lnf[:], in_=pbank[7][0:64, :], func=AF.Exp, scale=-1.0),
                              reads=[bP[7]], writes=[blnf])
                        S_.op(DVE, lambda e: e.tensor_tensor(out=t0[:], in0=pbank[pob][0:64, :], in1=lnf[:],
                                                             op=ALU.mult),
                              reads=[bP[pob], blnf], writes=[bt0])
                        S_.op(ACT, lambda e: e.activation(out=sq65[0:64, :], in_=t0[:], func=AF.Square),
                              reads=[bt0], writes=[bsq65])
                        yi = ycnt[0] % 2
                        ycnt[0] += 1
                        S_.op(DVE, lambda e: e.tensor_scalar(out=yf[yi][:], in0=t0[:], scalar1=ogc[:, h:h + 1],
                                                             scalar2=None, op0=ALU.mult),
                              reads=[bt0, bK], writes=[byf[yi]])
                        if gated_gi is not None:
                            S_.op(POOL, lambda e: e.tensor_tensor(out=yf[yi][:], in0=yf[yi][:],
                                                                  in1=gt[gated_gi][:], op=ALU.mult),
                                  reads=[bgt[gated_gi]], writes=[byf[yi]])
                        S_.dma(SP, lambda e: e.dma_start(out=YT[h * 64:(h + 1) * 64, cs], in_=yf[yi][:]),
                               byf[yi], store=True, reads=[byf[yi]], pwrites=[bYT])

                    def partC():
                        def fss(e):
                            ins = None
                            for i in range(4):
                                ins = e.matmul(pbank[7][:, i:i + 1], lhsT=sq65[0:64, i * 128:(i + 1) * 128],
                                               rhs=c65[:], start=True, stop=True)
                            return ins
                        S_.op(PE, fss, reads=[bsq65, bC], writes=[bP[7]])
                        S_.op(DVE, lambda e: e.tensor_tensor(out=SSQ[:, 4 * c:4 * c + 4, g],
                                                             in0=SSQ[:, 4 * c:4 * c + 4, g],
                                                             in1=pbank[7][:, 0:4], op=ALU.add),
                              reads=[bP[7]], pwrites=[bSSQ])
                    deferred.append([1, partA])
                    deferred.append([3, partB])
                    deferred.append([6, partC])

                def load_head(h, hi, kind):
                    S_.dma(SP, lambda e: e.dma_start(out=qa[hi][0:64, :], in_=QT[h * 64:(h + 1) * 64, :]),
                           bqa[hi], reads=[bQT], writes=[bqa[hi]])
                    S_.dma(SP, lambda e: e.dma_start(out=ka[hi][0:64, :], in_=KT[h * 64:(h + 1) * 64, :]),
                           bka[hi], reads=[bKT], writes=[bka[hi]])
                    if kind == "fox":
                        hf = h - 4
                        r = 64
                        S_.dma(SP, lambda e: e.dma_start(out=qa[hi][r:r + 3, :], in_=CUMA[hf]), bqa[hi],
                               reads=[bCA], pwrites=[bqa[hi]])
                        S_.dma(SP, lambda e: e.dma_start(out=ka[hi][r + 3:r + 6, :], in_=CUMB[hf]), bka[hi],
                               reads=[bCB], pwrites=[bka[hi]])
                    else:
                        r = 80 if kind == "moba" else 64
                        S_.dma(SP, lambda e: e.dma_start(out=qa[hi][r:r + 3, :], in_=cd["posA"][h]), bqa[hi],
                               pwrites=[bqa[hi]])
                        S_.dma(SP, lambda e: e.dma_start(out=ka[hi][r + 3:r + 6, :], in_=cd["posB"][h]), bka[hi],
                               pwrites=[bka[hi]])
                    S_.dma(SP, lambda e: e.dma_start(out=qa[hi][r + 3:r + 6, :], in_=cd["ones3"]), bqa[hi],
                           pwrites=[bqa[hi]])
                    S_.dma(SP, lambda e: e.dma_start(out=ka[hi][r:r + 3, :], in_=cd["ones3"]), bka[hi],
                           pwrites=[bka[hi]])
                    if kind == "moba":
                        S_.dma(SP, lambda e: e.dma_start(out=ka[hi][64:80, :], in_=cd["ind16"]), bka[hi],
                               pwrites=[bka[hi]])

                def load_v(h, vt, bvts_, d):
                    for r in range(d):
                        src = V.rearrange("(b p r) h e -> p b r h e", p=128, r=d)[:, :, r, h, :]
                        dst = vt[:].rearrange("p (b r) e -> p b r e", r=d)[:, :, r, :]
                        S_.dma(SP, (lambda e, src=src, dst=dst: e.dma_start(out=dst, in_=src)), bvts_[r], reads=[bV],
                               writes=[bvts_[r]])

                def moba_prelude_items(h, hi):
                    def head_part():
                        S_.op(DVE, lambda e: e.memset(kms[:], 0.0), writes=[bkm])
                        S_.op(DVE, lambda e: e.tensor_reduce(out=kms[:, 0:S // 256],
                                                             in_=ka[hi][0:64, :].rearrange("p (n k) -> p n k", k=256),
                                                             axis=AX.X, op=ALU.add),
                              reads=[bka[hi]], writes=[bkm])
                        S_.op(DVE, lambda e: e.tensor_copy(out=kmb[:], in_=kms[:]), writes=[bkm])

                    def p1(i):
                        zi = i % 3
                        sl = slice(i * 16, (i + 1) * 16)
                        gc = slice((i % 8) * 16, (i % 8) * 16 + 16)
                        gm, top8, t1, t2 = gm2[zi], top82[zi], t12[zi], t22[zi]
                        bgm, btop8, bt1, bt2 = bgm2[zi], btop82[zi], bt12[zi], bt22[zi]
                        S_.op(PE, lambda e: e.matmul(pbank[6][:, gc], lhsT=qa[hi][0:64, i * 128:(i + 1) * 128],
                                                     rhs=kmb[:], start=True, stop=True),
                              reads=[bqa[hi], bkm], writes=[bP[6]])
                        S_.op(DVE, lambda e: e.tensor_tensor(out=gm[:], in0=pbank[6][:, gc], in1=negm[:, sl],
                                                             op=ALU.add),
                              reads=[bP[6], bK], writes=[bgm])
                        S_.op(DVE, lambda e: e.max(out=top8[:], in_=gm[:]), reads=[bgm], writes=[btop8])
                        S_.op(DVE, lambda e: e.scalar_tensor_tensor(out=t1[:], in0=gm[:], scalar=top8[:, 2:3],
                                                                    in1=past[:, sl], op0=ALU.is_ge, op1=ALU.mult),
                              reads=[bgm, btop8, bK], writes=[bt1])
                        S_.op(DVE, lambda e: e.tensor_tensor(out=t2[:], in0=t1[:], in1=own[:, sl], op=ALU.add),
                              reads=[bt1, bK], writes=[bt2])
                        S_.op(DVE, lambda e: e.tensor_scalar(out=zp[zi][:, 64:80], in0=t2[:], scalar1=-1.0,
                                                             scalar2=-MASKNEG, op0=ALU.add, op1=ALU.mult),
                              reads=[bt2], pwrites=[bzp[zi]])

                    def p2(i):
                        zi = i % 3
                        mc = slice((i % 2) * 128, (i % 2) * 128 + 128)
                        S_.op(PE, lambda e: e.matmul(pbank[7][0:80, mc], lhsT=zp[zi][:, 0:80], rhs=ident[:],
                                                     start=True, stop=True),
                              reads=[bzp[zi], bC], writes=[bP[7]])
                        S_.op(ACT, lambda e: e.copy(out=qa[hi][64:80, i * 128:(i + 1) * 128], in_=pbank[7][64:80, mc]),
                              reads=[bP[7]], pwrites=[bqa[hi]])

                    def item(k):
                        def f():
                            if k - 2 >= 0:
                                p2(k - 2)
                            if k < NT:
                                p1(k)
                        return f
                    return [head_part] + [item(k) for k in range(NT + 2)]

                LOOK = 2
                deferred = []

                def tick():
                    keep = []
                    for item in deferred:
                        item[0] -= 1
                        if item[0] <= 0:
                            item[1]()
                        else:
                            keep.append(item)
                    deferred[:] = keep

                def flush():
                    while deferred:
                        tick()

                bg = []

                def run_pipeline(steps, LOOK=2):
                    n = len(steps)
                    for i in range(min(LOOK, n)):
                        steps[i][0]()
                    for k in range(n):
                        if k + LOOK < n:
                            steps[k + LOOK][0]()
                        steps[k][1]()
                        tick()
                        if bg and k % 4 == 1:
                            bg.pop(0)()
                    while bg:
                        bg.pop(0)()

                def causal_head(h, hi, kind):
                    R = 86 if kind == "moba" else 70
                    if debug and h == 0:
                        S_.dma(SP, lambda e: e.dma_start(out=DBGQ, in_=qa[hi][0:86, :]), bDBG, store=True,
                               reads=[bqa[hi]], pwrites=[bDBG])
                    steps = []
                    k = 0
                    for c in range(NCH):
                        for jt in range(4 * c + 4):
                            steps.append(mk_causal_step(h, hi, kind, R, c, jt, k))
                            k += 1
                    run_pipeline(steps, LOOK=3)

                def mk_causal_step(h, hi, kind, R, c, jt, k):
                    njt = 4 * c + 4
                    m = jt - 4 * c
                    off = max(m, 0) * 128
                    ps = k % 4
                    po = 4 + (c % 2)

                    def qk():
                        S_.op(PE, lambda e: e.matmul(pbank[ps][:, off:512], lhsT=ka[hi][0:R, jt * 128:(jt + 1) * 128],
                                                     rhs=qa[hi][0:R, c * 512 + off:(c + 1) * 512],
                                                     start=True, stop=True),
                              reads=[bqa[hi], bka[hi]], writes=[bP[ps]])

                    def rest():
                        if jt == 0 and kind == "fox":
                            gi = c % 2
                            S_.dma(SP, lambda e: e.dma_start(
                                out=gt[gi][:], in_=GT[(h - 4) * 64:(h - 3) * 64, c * 512:(c + 1) * 512]),
                                   bgt[gi], reads=[bGT], writes=[bgt[gi]])
                        if m >= 0:
                            S_.op(DVE, lambda e: e.tensor_tensor(
                                out=pbank[ps][:, off:off + 128], in0=pbank[ps][:, off:off + 128], in1=tri[:],
                                op=ALU.add), reads=[bC], writes=[bP[ps]])
                        S_.op(ACT, lambda e: e.activation(out=pT[ps][:, off:512], in_=pbank[ps][:, off:512],
                                                          func=AF.Exp),
                              reads=[bP[ps]], writes=[bpT[ps]])
                        S_.op(PE, lambda e: e.matmul(pbank[po][0:65, off:512], lhsT=va[hi][:, jt, :],
                                                     rhs=pT[ps][:, off:512], start=(jt == 0), stop=(jt == njt - 1)),
                              reads=[bpT[ps], bva[hi]], writes=[bP[po]] if jt == 0 else (),
                              pwrites=() if jt == 0 else [bP[po]])
                        if jt == njt - 1:
                            finalize(h, po, c, gated_gi=(c % 2) if kind == "fox" else None)
                    return (qk, rest)

                def dil_head(h, hi):
                    R = 70
                    dils = (1, 4, 16)
                    vts = (va[hi], vd[hi][0], vd[hi][1])
                    bvts = ([bva[hi]], bvd[hi][0], bvd[hi][1])
                    SC = min(2048, S)
                    nq = SC // 512
                    kk = [0]
                    for sc in range(S // SC):
                        flush()
                        for q in range(nq):
                            S_.op(PE, (lambda e, q=q: e.matmul(pbank[3 + q][0:65, :], lhsT=zero1[0:1, 0:65],
                                                               rhs=qa[hi][0:1, 0:512], start=True, stop=False)),
                                  reads=[bK, bqa[hi]], writes=[bP[3 + q]])
                        blocks = []
                        for di, d in enumerate(dils):
                            nblk = SC // (d * 128)
                            for r in range(d):
                                for b in range(sc * nblk, (sc + 1) * nblk):
                                    blocks.append((di, d, r, b))
                        steps = []
                        for i in range(0, len(blocks), 2):
                            steps.append(mk_dil_step(h, hi, R, sc, SC, nq, blocks[i:i + 2], kk[0], vts, bvts))
                            kk[0] += 1
                        run_pipeline(steps)
                        for q in range(nq):
                            S_.op(PE, (lambda e, q=q: e.matmul(pbank[3 + q][0:65, :], lhsT=zero1[0:1, 0:65],
                                                               rhs=qa[hi][0:1, 0:512], start=False, stop=True)),
                                  reads=[bK, bqa[hi]], pwrites=[bP[3 + q]])
                        for q in range(nq):
                            finalize(h, 3 + q, (sc * SC) // 512 + q)
                            flush()

                def mk_dil_step(h, hi, R, sc, SC, nq, blks, k, vts, bvts):
                    ps = k % 3
                    W = 256 * len(blks)

                    def qk():
                        def f(e):
                            ins = None
                            for i, (di, d, r, b) in enumerate(blks):
                                qv = qa[hi][0:R, :].rearrange("p (i d) -> p d i", d=d)
                                kv = ka[hi][0:R, :].rearrange("p (i d) -> p d i", d=d)
                                for s_, kb in ((0, b - 1), (1, b)):
                                    if kb < 0:
                                        ins = e.matmul(pbank[ps][:, i * 256:i * 256 + 128], lhsT=zero1[0:1, :],
                                                       rhs=qv[0:1, r, b * 128:(b + 1) * 128], start=True, stop=True)
                                        continue
                                    ins = e.matmul(pbank[ps][:, i * 256 + s_ * 128:i * 256 + (s_ + 1) * 128],
                                                   lhsT=kv[:, r, kb * 128:(kb + 1) * 128],
                                                   rhs=qv[:, r, b * 128:(b + 1) * 128], start=True, stop=True)
                            return ins
                        S_.op(PE, f, reads=[bqa[hi], bka[hi], bK], writes=[bP[ps]])

                    def rest():
                        S_.op(DVE, lambda e: e.tensor_tensor(out=pbank[ps][:, 0:W], in0=pbank[ps][:, 0:W],
                                                             in1=band[:, 0:W], op=ALU.add),
                              reads=[bC], writes=[bP[ps]])
                        S_.op(ACT, lambda e: e.activation(out=pT[ps][:, 0:W], in_=pbank[ps][:, 0:W], func=AF.Exp),
                              reads=[bP[ps]], writes=[bpT[ps]])
                        banks = set()
                        plan = []
                        for i, (di, d, r, b) in enumerate(blks):
                            tstart = r + d * b * 128 - sc * SC
                            span = d * 128
                            segs = []
                            if span <= 512:
                                q = tstart // 512
                                base = tstart - q * 512
                                if d == 1:
                                    oview = pbank[3 + q][0:65, base:base + 128]
                                else:
                                    oview = pbank[3 + q][0:65, :].rearrange("p (u d) -> p d u", d=d)[:, r, :]
                                segs.append((oview, 0, 128))
                                banks.add(3 + q)
                            else:
                                per = 512 // d
                                for q in range(nq):
                                    oview = pbank[3 + q][0:65, :].rearrange("p (u d) -> p d u", d=d)[:, r, :]
                                    segs.append((oview, q * per, per))
                                    banks.add(3 + q)
                            slots = [(1, b)] if b == 0 else [(0, b - 1), (1, b)]
                            plan.append((i, di, d, r, slots, segs))

                        def fpv(e):
                            ins = None
                            for (i, di, d, r, slots, segs) in plan:
                                for (s_, kb) in slots:
                                    for (oview, u0, n) in segs:
                                        c0 = i * 256 + s_ * 128 + u0
                                        ins = e.matmul(oview, lhsT=vts[di][:, kb * d + r, :], rhs=pT[ps][:, c0:c0 + n],
                                                       start=False, stop=False)
                            return ins
                        S_.op(PE, fpv, reads=[bpT[ps]] + [bvts[p[1]][p[3] if p[1] > 0 else 0] for p in plan],
                              pwrites=[bP[x] for x in sorted(banks)])
                    return (qk, rest)

                heads = [(h, "fox") for h in range(4, 10)] + [(h, "moba") for h in range(0, 4)] + \
                        [(h, "dil") for h in range(10, 16)]
                def load_all(idx):
                    h, kind = heads[idx]
                    hi = idx % 2
                    load_head(h, hi, kind)
                    load_v(h, va[hi], [bva[hi]], 1)
                    if kind == "dil":
                        load_v(h, vd[hi][0], bvd[hi][0], 4)
                        load_v(h, vd[hi][1], bvd[hi][1], 16)

                load_all(0)
                for idx, (h, kind) in enumerate(heads):
                    hi = idx % 2
                    if idx + 1 < len(heads):
                        load_all(idx + 1)
                    if kind == "moba" and idx == 0:
                        for it in moba_prelude_items(h, hi):
                            it()
                    if idx + 1 < len(heads) and heads[idx + 1][1] == "moba":
                        bg.extend(moba_prelude_items(heads[idx + 1][0], (idx + 1) % 2))
                    if kind == "dil":
                        dil_head(h, hi)
                    else:
                        causal_head(h, hi, kind)
                flush()
                for g, W in ((0, 256), (1, 384), (2, 384)):
                    S_.op(ACT, (lambda e, g=g, W=W: e.activation(out=RG[:, :, g], in_=SSQ[:, :, g], func=AF.Ln,
                                                                 bias=epsb[:], scale=1.0 / W)),
                          reads=[bSSQ, bC], pwrites=[bRG])
                    S_.op(ACT, (lambda e, g=g: e.activation(out=RG[:, :, g], in_=RG[:, :, g], func=AF.Exp,
                                                            scale=-0.5)), pwrites=[bRG])
            S_.barrier()

        def load_w3a(l, es):
            wo = sbt(es, "wo", [128, 8, D], BF16)
            wu = sbt(es, "wu", [128, 8, DFF], BF16)
            bwo = [Buf("wo%d" % i) for i in range(2)]
            bwu = [Buf("wu%d" % i) for i in range(4)]
            for i in range(2):
                S_.dma(POOL, (lambda e, i=i: e.dma_start(
                    out=wo[:, 4 * i:4 * i + 4, :],
                    in_=w_out[l, 512 * i:512 * (i + 1), :].rearrange("(k p) n -> p k n", p=128))),
                       bwo[i], writes=[bwo[i]])
            for i in range(4):
                S_.dma(POOL, (lambda e, i=i: e.dma_start(
                    out=wu[:, 2 * i:2 * i + 2, :],
                    in_=w_up[l, 256 * i:256 * (i + 1), :].rearrange("(k p) n -> p k n", p=128))),
                       bwu[i], writes=[bwu[i]])
            return wo, wu, bwo, bwu

        def phase3(l, x_src, bx_src, x_dst, bx_dst, w3a):
            TC = 256
            wo, wu, bwo, bwu = w3a
            with ExitStack() as es:
                wd = sbt(es, "wd", [128, 32, D], BF16)
                bwd = [Buf("wd%d" % i) for i in range(4)]
                for i in range(4):
                    S_.dma(POOL, (lambda e, i=i: e.dma_start(
                        out=wd[:, 8 * i:8 * i + 8, :],
                        in_=w_down[l, 1024 * i:1024 * (i + 1), :].rearrange("(k p) n -> p k n", p=128))),
                           bwd[i], writes=[bwd[i]])
                g2 = sbt(es, "g2", [128, D], F32)
                bG = Buf("p3consts")
                S_.dma(SP, lambda e: e.dma_start(out=g2[:], in_=mlp_norm[l:l + 1, :].partition_broadcast(128)),
                       bG, pwrites=[bG])
                yT = [sbt(es, "yT%d" % i, [128, 8, TC], BF16) for i in range(2)]
                byT = [Buf("yT%d" % i) for i in range(2)]
                xt = [sbt(es, "x3_%d" % i, [128, D], F32) for i in range(4)]
                bxt = [Buf("x3_%d" % i) for i in range(4)]
                st4 = [sbt(es, "st43_%d" % i, [128, 4], F32) for i in range(2)]
                bst = [Buf("st43_%d" % i) for i in range(2)]
                xs = [sbt(es, "xs3_%d" % i, [128, D], BF16) for i in range(2)]
                bxs = [Buf("xs3_%d" % i) for i in range(2)]
                hnT = [sbt(es, "hn2T%d" % i, [128, 8, TC], BF16) for i in range(2)]
                bhn = [Buf("hn2T%d" % i) for i in range(2)]
                hT = sbt(es, "hT", [128, 32, TC], BF16)
                bhT = Buf("hT")
                rl = [sbt(es, "rl%d" % i, [128, 2 * TC], BF16) for i in range(2)]
                brl = [Buf("rl%d" % i) for i in range(2)]
                NC3 = S // TC
                NTT = TC // 128

                def loadY(c):
                    cb = c % 2
                    S_.dma(SP, (lambda e: e.dma_start(out=yT[cb][:], in_=YT.rearrange("(k p) s -> p k s", p=128)[:, :, c * TC:(c + 1) * TC])),
                           byT[cb], reads=[bYT], writes=[byT[cb]])

                def stageA1(c):
                    cb = c % 2
                    for tt in range(NTT):
                        k4 = (c * NTT + tt) % 4
                        t0 = c * TC + tt * 128
                        S_.dma(SP, (lambda e, k4=k4, t0=t0: e.dma_start(out=xt[k4][:], in_=x_src[t0:t0 + 128, :])),
                               bxt[k4], reads=[bx_src], writes=[bxt[k4]])
                    for tt in range(NTT):
                        k4 = (c * NTT + tt) % 4
                        k = tt % 2
                        t0 = c * TC + tt * 128
                        ti = t0 // 128
                        for half in range(2):
                            for g, kcs in ((0, (0, 1)), (1, (2, 3, 4)), (2, (5, 6, 7))):
                                pb = (half * 3 + g) % 2

                                def f(e, half=half, tt=tt, kcs=kcs, pb=pb):
                                    ins = None
                                    for kc in kcs:
                                        ins = e.matmul(pbank[pb][:], lhsT=yT[cb][:, kc, tt * 128:(tt + 1) * 128],
                                                       rhs=wo[:, kc, half * 512:(half + 1) * 512],
                                                       start=(kc == kcs[0]), stop=(kc == kcs[-1]))
                                    return ins
                                S_.op(PE, f, reads=[byT[cb]] + bwo, writes=[bP[pb]])
                                S_.op(DVE, (lambda e, half=half, k4=k4, pb=pb, g=g, ti=ti: e.scalar_tensor_tensor(
                                    out=xt[k4][:, half * 512:(half + 1) * 512], in0=pbank[pb][:],
                                    scalar=RG[:, ti, g:g + 1], in1=xt[k4][:, half * 512:(half + 1) * 512],
                                    op0=ALU.mult, op1=ALU.add)),
                                      reads=[bP[pb], bRG], writes=[bxt[k4]])
                        if debug and XM is not None:
                            S_.dma(SP, (lambda e, k4=k4, t0=t0: e.dma_start(out=XM[t0:t0 + 128, :], in_=xt[k4][:])),
                                   bxt[k4], store=True, reads=[bxt[k4]], pwrites=[bXM])
                        S_.op(ACT, (lambda e, k=k, k4=k4: e.activation(out=xs[k][:], in_=xt[k4][:], func=AF.Square)),
                              reads=[bxt[k4]], writes=[bxs[k]])
                        S_.op(DVE, (lambda e, k=k: e.tensor_reduce(out=st4[k][:, 0:1], in_=xs[k][:], axis=AX.X,
                                                                   op=ALU.add)),
                              reads=[bxs[k]], writes=[bst[k]])
                        S_.op(ACT, (lambda e, k=k: e.activation(out=st4[k][:, 1:2], in_=st4[k][:, 0:1], func=AF.Ln,
                                                                bias=epsb[:], scale=1.0 / D)),
                              reads=[bC], writes=[bst[k]])
                        S_.op(ACT, (lambda e, k=k: e.activation(out=st4[k][:, 2:3], in_=st4[k][:, 1:2], func=AF.Exp,
                                                                scale=-0.5)), writes=[bst[k]])
                        S_.op(DVE, (lambda e, k=k, k4=k4: e.scalar_tensor_tensor(out=xs[k][:], in0=xt[k4][:],
                                                                                 scalar=st4[k][:, 2:3], in1=g2[:],
                                                                                 op0=ALU.mult, op1=ALU.mult)),
                              reads=[bxt[k4], bst[k], bG], writes=[bxs[k]])

                def stageA2(c):
                    cb = c % 2
                    for tt in range(NTT):
                        k = tt % 2
                        pbf = pbank[2][:].bitcast(BF16)

                        def tr(e, k=k, pbf=pbf):
                            ins = None
                            for dc in range(8):
                                ins = e.transpose(pbf[:, dc * 128:(dc + 1) * 128], xs[k][:, dc * 128:(dc + 1) * 128],
                                                  ident[:])
                            return ins
                        S_.op(PE, tr, reads=[bxs[k], bC], writes=[bP[2]])
                        S_.op(ACT, (lambda e, tt=tt, pbf=pbf: e.copy(
                            out=hnT[cb][:, :, tt * 128:(tt + 1) * 128],
                            in_=pbf.rearrange("p (c t) -> p c t", c=8))),
                              reads=[bP[2]], pwrites=[bhn[cb]])

                def stageBup(c):
                    cb = c % 2
                    for f2 in range(16):
                        pu = 2 + (f2 % 2)
                        ri = f2 % 2

                        def f(e, f2=f2, pu=pu):
                            ins = None
                            for s in range(2):
                                fc = f2 * 2 + s
                                for dc in range(8):
                                    ins = e.matmul(pbank[pu][:, s * TC:(s + 1) * TC],
                                                   lhsT=wu[:, dc, fc * 128:(fc + 1) * 128], rhs=hnT[cb][:, dc, :],
                                                   start=(dc == 0), stop=(dc == 7))
                            return ins
                        S_.op(PE, f, reads=bwu + [bhn[cb]], writes=[bP[pu]])
                        S_.op(ACT, (lambda e, pu=pu, ri=ri: e.activation(out=rl[ri][:], in_=pbank[pu][:],
                                                                         func=AF.Relu)),
                              reads=[bP[pu]], writes=[brl[ri]])
                        eng = POOL if f2 % 2 else DVE
                        S_.op(eng, (lambda e, f2=f2, ri=ri: e.tensor_tensor(
                            out=hT[:, 2 * f2:2 * f2 + 2, :], in0=rl[ri][:].rearrange("p (s t) -> p s t", s=2),
                            in1=rl[ri][:].rearrange("p (s t) -> p s t", s=2), op=ALU.mult)),
                              reads=[brl[ri]], pwrites=[bhT])

                def stageBdown(c):
                    def f(e):
                        ins = None
                        for fc in range(32):
                            for tt in range(NTT):
                                for half in range(2):
                                    ins = e.matmul(pbank[4 + tt * 2 + half][:],
                                                   lhsT=hT[:, fc, tt * 128:(tt + 1) * 128],
                                                   rhs=wd[:, fc, half * 512:(half + 1) * 512],
                                                   start=(fc == 0), stop=(fc == 31))
                        return ins
                    S_.op(PE, f, reads=[bhT] + bwd, writes=[bP[4 + i] for i in range(2 * NTT)])
                    for tt in range(NTT):
                        k4 = (c * NTT + tt) % 4
                        t0 = c * TC + tt * 128
                        for half in range(2):
                            S_.op(DVE, (lambda e, tt=tt, half=half, k4=k4: e.tensor_tensor(
                                out=xt[k4][:, half * 512:(half + 1) * 512], in0=pbank[4 + tt * 2 + half][:],
                                in1=xt[k4][:, half * 512:(half + 1) * 512], op=ALU.add)),
                                  reads=[bP[4 + tt * 2 + half]], writes=[bxt[k4]])
                        S_.dma(SP, (lambda e, k4=k4, t0=t0: e.dma_start(out=x_dst[t0:t0 + 128, :], in_=xt[k4][:])),
                               bxt[k4], store=True, reads=[bxt[k4]], pwrites=[bx_dst])

                loadY(0)
                loadY(1)
                stageA1(0)
                stageA2(0)
                for c in range(NC3):
                    if c + 1 < NC3:
                        stageA1(c + 1)
                    if c + 2 < NC3:
                        loadY(c + 2)
                    stageBup(c)
                    if c + 1 < NC3:
                        stageA2(c + 1)
                    stageBdown(c)
            S_.barrier()

        bxin = Buf("x_in")
        for l in range(NL):
            x_src, bsrc = (x_in, bxin) if l == 0 else (X1, bX1)
            x_dst, bdst = (out, bOUT) if l == NL - 1 else (X1, bX1)
            phase1(l, x_src, bsrc)
            with ExitStack() as wes:
                w3a = load_w3a(l, wes)
                phase2(l)
                phase3(l, x_src, bsrc, x_dst, bdst, w3a)
        S_.emit(final_waits=_compress(bOUT.writers))
        S_.close()
    return nc, consts_np


def _prep_weights(inp, NL):
    f = lambda a: np.ascontiguousarray(np.asarray(a, dtype=np.float32))
    qg = f(inp["q_gain"])
    kg = f(inp["k_gain"])
    og = f(inp["out_gain"])
    m = {
        "attn_norm": f(inp["attn_norm"])[:NL],
        "w_in": f(inp["w_in"])[:NL],
        "b_forget": f(inp["b_forget"])[:NL].reshape(NL, N_FOX, 1),
        "qg_col": np.ascontiguousarray(qg[:NL].reshape(NL, 8, 2, 64).transpose(0, 2, 3, 1).reshape(NL, 128, 8)),
        "kg_col": np.ascontiguousarray(kg[:NL].reshape(NL, 8, 2, 64).transpose(0, 2, 3, 1).reshape(NL, 128, 8)),
        "og_col": np.ascontiguousarray(og[:NL].reshape(NL, 16, 64).transpose(0, 2, 1)),
        "w_out": f(inp["w_out"])[:NL],
        "mlp_norm": f(inp["mlp_norm"])[:NL],
        "w_up": f(inp["w_up"])[:NL],
        "w_down": f(inp["w_down"])[:NL],
    }
    return m


_CACHE = {}


def kernel(**inputs):
    x = np.asarray(inputs["x"], dtype=np.float32)
    B, S, _ = x.shape
    NL = 2
    key = (S, NL)
    if key not in _CACHE:
        _CACHE[key] = build(S, NL)
    nc, consts = _CACHE[key]
    wm = _prep_weights(inputs, NL)
    for k, v in consts.items():
        wm["c_" + k] = v
    in_maps = []
    for b in range(B):
        m = dict(wm)
        m["x"] = np.ascontiguousarray(x[b])
        in_maps.append(m)
    res = run_bass_kernel_spmd(nc, in_maps, core_ids=list(range(B)))
    return np.stack([np.asarray(r["out"], dtype=np.float32) for r in res.results], axis=0)
```

```python
from contextlib import ExitStack
import numpy as np
import ml_dtypes
import concourse.bass as bass
import concourse.mybir as mybir
from concourse.bass_utils import run_bass_kernel_spmd

F32 = mybir.dt.float32
BF16 = mybir.dt.bfloat16
ALU = mybir.AluOpType
AF = mybir.ActivationFunctionType
AX = mybir.AxisListType

PE, ACT, DVE, POOL, SP = "tensor", "scalar", "vector", "gpsimd", "sync"
ENGS = (PE, ACT, DVE, POOL, SP)

D = 1024
NH = 16
DH = 64
NCOL = 3462
DFF = 4096
EPS = 1e-6
N_FOX = 6
MASKNEG = -1024.0


class Op:
    __slots__ = ("eng", "fn", "deps", "signal", "sem", "val", "is_dma")


class Buf:
    def __init__(self, name):
        self.name = name
        self.writers = []
        self.readers = []
        self.ld = None
        self.st = None


def _compress(lst):
    best = {}
    for o in lst:
        key = o.sem if o.is_dma else o.eng
        cur = best.get(key)
        if cur is None or o.val > cur.val:
            best[key] = o
    return list(best.values())


class Sched:
    def __init__(self, nc):
        self.nc = nc
        self.ops = {e: [] for e in ENGS}
        self.seq = 0
        self._semctx = []
        self.pool = {}
        self.live = []
        self.last_compute = {}
        self.dma_latest = {}
        self.engsem = {e: self.new_sem("prog_" + e) for e in ENGS}

    def new_sem(self, name):
        ctx = self.nc.semaphore(name)
        s = ctx.__enter__()
        self._semctx.append(ctx)
        return s

    def close(self):
        for ctx in reversed(self._semctx):
            ctx.__exit__(None, None, None)

    def _mk(self, eng, fn, reads, writes, pwrites):
        o = Op()
        o.eng, o.fn, o.signal, o.sem, o.is_dma = eng, fn, False, None, False
        self.seq += 1
        o.val = self.seq
        deps = []
        for b in reads:
            deps.extend(b.writers)
        for b in writes:
            deps.extend(b.writers)
            deps.extend(b.readers)
        for b in pwrites:
            deps.extend(b.writers)
            deps.extend(b.readers)
        o.deps = deps
        return o

    def _commit(self, o, reads, writes, pwrites):
        o.deps = _compress(o.deps)
        for b in reads:
            b.readers.append(o)
            if len(b.readers) > 12:
                b.readers = _compress(b.readers)
        for b in writes:
            b.writers = [o]
            b.readers = []
        for b in pwrites:
            b.writers.append(o)
            if len(b.writers) > 12:
                b.writers = _compress(b.writers)
        self.ops[o.eng].append(o)
        if o.is_dma:
            self.dma_latest[id(o.sem)] = o
        else:
            self.last_compute[o.eng] = o

    def op(self, eng, fn, reads=(), writes=(), pwrites=()):
        o = self._mk(eng, fn, reads, writes, pwrites)
        self._commit(o, reads, writes, pwrites)
        return o

    def _slot(self, sb, store, eng):
        slot = sb.st if store else sb.ld
        if slot is None:
            pool = self.pool.setdefault(eng, [])
            slot = pool.pop() if pool else [self.new_sem("dma%d" % len(self._semctx)), 0, None, eng]
            if store:
                sb.st = slot
            else:
                sb.ld = slot
            self.live.append(sb)
        return slot

    def dma(self, eng, fn, sb, store=False, reads=(), writes=(), pwrites=()):
        o = self._mk(eng, fn, reads, writes, pwrites)
        o.is_dma = True
        slot = self._slot(sb, store, eng)
        slot[1] += 16
        o.sem, o.val = slot[0], slot[1]
        if len(slot) > 2 and slot[2] is not None:
            o.deps.append(slot[2])
        if len(slot) > 2:
            slot[2] = o
        else:
            slot.append(o)
        self._commit(o, reads, writes, pwrites)
        return o

    def barrier(self):
        deps = list(self.last_compute.values()) + list(self.dma_latest.values())
        for e in ENGS:
            o = Op()
            o.eng, o.fn, o.signal, o.sem, o.is_dma = e, (lambda eng: None), False, None, False
            self.seq += 1
            o.val = self.seq
            o.deps = list(deps)
            self.ops[e].append(o)
        for b in self.live:
            if b.ld is not None:
                self.pool.setdefault(b.ld[3], []).append(b.ld)
                b.ld = None
            if b.st is not None:
                self.pool.setdefault(b.st[3], []).append(b.st)
                b.st = None
        self.live = []

    def emit(self, final_waits=()):
        nc = self.nc
        for e in ENGS:
            for o in self.ops[e]:
                for d in o.deps:
                    if not d.is_dma and (d.eng != o.eng or o.is_dma or d.eng != PE):
                        d.signal = True
        for e in ENGS:
            c = 0
            for o in self.ops[e]:
                if not o.is_dma and o.signal:
                    c += 1
                    o.val = c
                    o.sem = self.engsem[e]
        ops = self.ops

        def run(eng_name, e):
            waited = {}
            for o in ops[eng_name]:
                need = {}
                for d in o.deps:
                    if not d.is_dma and d.eng == o.eng and not o.is_dma and d.eng == PE:
                        continue
                    k = id(d.sem)
                    if k not in need or need[k][1] < d.val:
                        need[k] = (d.sem, d.val)
                for k, (sem, v) in need.items():
                    if waited.get(k, 0) < v:
                        e.wait_ge(sem, v)
                        waited[k] = v
                ins = o.fn(e)
                if ins is None:
                    continue
                if o.is_dma:
                    ins.then_inc(o.sem, 16)
                elif o.signal:
                    ins.then_inc(o.sem, 1)
            if eng_name == SP:
                for o in final_waits:
                    e.wait_ge(o.sem, o.val)

        with nc.allow_low_precision(reason="bf16 matmul operands are produced by fp32 math"), nc.Block() as block:
            @block.sync
            def _(e):
                run(SP, e)

            @block.tensor
            def _(e):
                run(PE, e)

            @block.scalar
            def _(e):
                run(ACT, e)

            @block.vector
            def _(e):
                run(DVE, e)

            @block.gpsimd
            def _(e):
                run(POOL, e)


def _split3(a):
    a = a.astype(np.float32)
    h = a.astype(ml_dtypes.bfloat16)
    r1 = a - h.astype(np.float32)
    m = r1.astype(ml_dtypes.bfloat16)
    r2 = r1 - m.astype(np.float32)
    lo = r2.astype(ml_dtypes.bfloat16)
    return np.stack([h, m, lo], axis=-2)


def make_consts(S):
    bf = ml_dtypes.bfloat16
    NT = S // 128
    c = {}
    c["ident"] = np.eye(128, dtype=np.float32).astype(bf)
    j = np.arange(128)[:, None]
    t = np.arange(128)[None, :]
    c["tri"] = np.where(j <= t, 0.0, -30000.0).astype(np.float32)
    band = np.zeros((128, 256), np.float32)
    band[:, 0:128] = np.where(j >= t, 0.0, -30000.0)
    band[:, 128:256] = np.where(j <= t, 0.0, -30000.0)
    c["band"] = np.concatenate([band, band], axis=1).astype(np.float32)
    onesf = np.zeros((65, 64), np.float32)
    onesf[64, :] = 1.0
    c["onesf"] = onesf
    blk = np.zeros((128, 128), np.float32)
    blk[0:64, 0:64] = 1.0 / 64
    blk[64:128, 64:128] = 1.0 / 64
    c["blk"] = blk.astype(bf)
    c["c65"] = np.ones((64, 1), np.float32).astype(bf)
    slopes = np.exp2(-8.0 * np.arange(1, 11, dtype=np.float32) / 10).astype(np.float32)
    hs = np.zeros(16, np.float32)
    hs[0:4] = slopes[6:10]
    hs[10:16] = slopes[0:6]
    pos = (hs[:, None] * np.arange(S, dtype=np.float32)[None, :]).astype(np.float32)
    c["posB"] = _split3(pos)
    c["posA"] = (-c["posB"].astype(np.float32)).astype(bf)
    c["ones3"] = np.ones((3, S), np.float32).astype(bf)
    n = np.arange(16)[:, None]
    c["ind16"] = ((np.arange(S)[None, :] // 256) == n).astype(np.float32).astype(bf)
    qb = (np.arange(NT) // 2)[:, None]
    nn = np.arange(16)[None, :]
    negm = np.where(nn < qb, 0.0, -1e30).astype(np.float32).reshape(1, NT * 16)
    past = (nn < qb).astype(np.float32).reshape(1, NT * 16)
    own = (nn == qb).astype(np.float32).reshape(1, NT * 16)
    c["negm"] = np.ascontiguousarray(np.broadcast_to(negm, (128, NT * 16)))
    c["past"] = np.ascontiguousarray(np.broadcast_to(past, (128, NT * 16)))
    c["own"] = np.ascontiguousarray(np.broadcast_to(own, (128, NT * 16)))
    return c


CONST_DT = {"ident": BF16, "tri": F32, "band": F32, "blk": BF16, "c65": BF16, "onesf": F32, "posA": BF16,
            "posB": BF16, "ones3": BF16, "ind16": BF16, "negm": F32, "past": F32, "own": F32}


def build(S=4096, NL=2, debug=False):
    NT = S // 128
    NCH = S // 512
    nc = bass.Bass("TRN2", target_bir_lowering=False)
    consts_np = make_consts(S)

    def din(name, shape, dt=F32):
        return nc.dram_tensor(name, list(shape), dt, kind="ExternalInput").ap()

    x_in = din("x", [S, D])
    attn_norm = din("attn_norm", [NL, D])
    w_in = din("w_in", [NL, D, NCOL])
    negb_in = din("b_forget", [NL, N_FOX, 1])
    qg_col = din("qg_col", [NL, 128, 8])
    kg_col = din("kg_col", [NL, 128, 8])
    og_col = din("og_col", [NL, 64, 16])
    w_out = din("w_out", [NL, D, D])
    mlp_norm = din("mlp_norm", [NL, D])
    w_up = din("w_up", [NL, D, DFF])
    w_down = din("w_down", [NL, DFF, D])
    cd = {k: din("c_" + k, v.shape, CONST_DT[k]) for k, v in consts_np.items()}
    out = nc.dram_tensor("out", [S, D], F32, kind="ExternalOutput").ap()
    skind = "ExternalOutput" if debug else "Internal"

    def dscr(name, shape, dt):
        return nc.dram_tensor(name, list(shape), dt, kind=skind).ap()

    QT = dscr("QT", [D, S], BF16)
    KT = dscr("KT", [D, S], BF16)
    V = dscr("V", [S, NH, 65], BF16)
    GT = dscr("GT", [N_FOX * 64, S], BF16)
    CUMA = dscr("CUMA", [N_FOX, 3, S], BF16)
    CUMB = dscr("CUMB", [N_FOX, 3, S], BF16)
    YT = dscr("YT", [D, S], BF16)
    X1 = dscr("X1", [S, D], F32)
    XM = dscr("XM", [S, D], F32) if debug else None
    DBGQ = dscr("DBGQ", [86, S], BF16) if debug else None
    bDBG = Buf("DBG")

    S_ = Sched(nc)
    bQT, bKT, bV, bGT, bCA, bCB, bYT, bX1, bOUT = [Buf(n) for n in
                                                   ("QT", "KT", "V", "GT", "CA", "CB", "YT", "X1", "OUT")]
    bXM = Buf("XM")

    with ExitStack() as top:
        uid = [0]

        def sbt(es, name, shape, dt):
            uid[0] += 1
            return es.enter_context(nc.sbuf_tensor("%s_u%d" % (name, uid[0]), list(shape), dt))

        pbank = [top.enter_context(nc.psum_tensor("pb%d" % i, [128, 512], F32)) for i in range(8)]
        bP = [Buf("pb%d" % i) for i in range(8)]

        ident = sbt(top, "ident", [128, 128], BF16)
        tri = sbt(top, "tri", [128, 128], F32)
        band = sbt(top, "band", [128, 512], F32)
        blk = sbt(top, "blk", [128, 128], BF16)
        c65 = sbt(top, "c65", [64, 1], BF16)
        onesf = sbt(top, "onesf", [65, 64], F32)
        SSQ = sbt(top, "SSQ", [128, NT, 3], F32)
        RG = sbt(top, "RG", [128, NT, 3], F32)
        bSSQ = Buf("SSQ")
        bRG = Buf("RG")
        epsb = sbt(top, "epsb", [128, 1], F32)
        oneb = sbt(top, "oneb", [128, 1], F32)
        qsb = sbt(top, "qsb", [128, 1], F32)
        zerob = sbt(top, "zerob", [128, 1], F32)
        bC = Buf("consts")
        for nm, t_ in (("ident", ident), ("tri", tri), ("band", band), ("blk", blk), ("c65", c65), ("onesf", onesf)):
            S_.dma(SP, (lambda e, t_=t_, nm=nm: e.dma_start(out=t_[:], in_=cd[nm])), bC, pwrites=[bC])
        S_.op(POOL, lambda e: e.memset(epsb[:], EPS), pwrites=[bC])
        S_.op(POOL, lambda e: e.memset(oneb[:], 1.0), pwrites=[bC])
        S_.op(POOL, lambda e: e.memset(qsb[:], float(-np.log(8.0))), pwrites=[bC])
        S_.op(POOL, lambda e: e.memset(zerob[:], 0.0), pwrites=[bC])

        def phase1(l, x_src, bx_src):
            with ExitStack() as es:
                w_sb = sbt(es, "w_in_sb", [128, 8, NCOL], BF16)
                bWs = [Buf("w_in%d" % i) for i in range(4)]
                for i in range(4):
                    S_.dma(POOL, (lambda e, i=i: e.dma_start(
                        out=w_sb[:, 2 * i:2 * i + 2, :],
                        in_=w_in[l, 256 * i:256 * (i + 1), :].rearrange("(k p) n -> p k n", p=128))),
                           bWs[i], writes=[bWs[i]])
                g1 = sbt(es, "g1", [128, D], F32)
                gq = sbt(es, "gq", [128, 16], F32)
                negb = sbt(es, "negb", [N_FOX, 1], F32)
                ones6 = sbt(es, "ones6", [N_FOX, 512], F32)
                bG = Buf("p1consts")
                S_.dma(SP, lambda e: e.dma_start(out=g1[:], in_=attn_norm[l:l + 1, :].partition_broadcast(128)),
                       bG, pwrites=[bG])
                S_.dma(SP, lambda e: e.dma_start(out=gq[:, 0:8], in_=qg_col[l]), bG, pwrites=[bG])
                S_.dma(SP, lambda e: e.dma_start(out=gq[:, 8:16], in_=kg_col[l]), bG, pwrites=[bG])
                S_.dma(SP, lambda e: e.dma_start(out=negb[:], in_=negb_in[l]), bG, pwrites=[bG])
                S_.op(DVE, lambda e: e.tensor_scalar(out=negb[:], in0=negb[:], scalar1=-1.0, scalar2=None,
                                                     op0=ALU.mult), reads=[bG], pwrites=[bG])
                S_.op(POOL, lambda e: e.memset(ones6[:], 1.0), pwrites=[bG])

                xt = [sbt(es, "xt%d" % i, [128, D], F32) for i in range(4)]
                bxt = [Buf("xt%d" % i) for i in range(4)]
                st8 = sbt(es, "st8", [128, 8], F32)
                bss = [Buf("ss%d" % i) for i in range(4)]
                brs = Buf("rs")
                st4 = [sbt(es, "st4_%d" % i, [128, 4], F32) for i in range(2)]
                bst = [Buf("st4_%d" % i) for i in range(2)]
                xs = [sbt(es, "xs%d" % i, [128, D], BF16) for i in range(4)]
                bxs = [Buf("xs%d" % i) for i in range(4)]
                hnT = [sbt(es, "hnT%d" % i, [128, 8, 512], BF16) for i in range(2)]
                bhn = [Buf("hnT%d" % i) for i in range(2)]
                sq = [sbt(es, "sq%d" % i, [128, 512], BF16) for i in range(2)]
                bsq = [Buf("sq%d" % i) for i in range(2)]
                lnt = [sbt(es, "lnt%d" % i, [128, 512], F32) for i in range(2)]
                blnt = [Buf("lnt%d" % i) for i in range(2)]
                qo = [sbt(es, "qo%d" % i, [128, 512], BF16) for i in range(3)]
                bqo = [Buf("qo%d" % i) for i in range(3)]
                vst = [sbt(es, "vst%d" % i, [128, NH, 65], BF16) for i in range(2)]
                bvst = [Buf("vst%d" % i) for i in range(2)]
                for i in range(2):
                    S_.op(POOL, (lambda e, i=i: e.memset(vst[i][:, :, 64:65], 1.0)), pwrites=[bvst[i]])
                cn = [sbt(es, "cn%d" % i, [N_FOX, 512], F32) for i in range(2)]
                bcn = [Buf("cn%d" % i) for i in range(2)]
                fr = sbt(es, "fr", [N_FOX, 512], F32)
                bfr = Buf("fr")
                fr2 = sbt(es, "fr2", [N_FOX, 512], F32)
                bfr2 = Buf("fr2")
                pa3 = [sbt(es, "pa3_%d" % i, [N_FOX, 3, 512], BF16) for i in range(2)]
                pb3 = [sbt(es, "pb3_%d" % i, [N_FOX, 3, 512], BF16) for i in range(2)]
                bp3 = [Buf("p3_%d" % i) for i in range(2)]

                def loadX(c):
                    for tt in range(4):
                        t0 = c * 512 + tt * 128
                        S_.dma(SP, (lambda e, tt=tt, t0=t0: e.dma_start(out=xt[tt][:], in_=x_src[t0:t0 + 128, :])),
                               bxt[tt], reads=[bx_src], writes=[bxt[tt]])

                def stageA1(c):
                    for tt in range(4):
                        S_.op(ACT, (lambda e, tt=tt: e.activation(out=xs[tt][:], in_=xt[tt][:], func=AF.Square)),
                              reads=[bxt[tt]], writes=[bxs[tt]])
                    for tt in range(4):
                        S_.op(DVE, (lambda e, tt=tt: e.tensor_reduce(out=st8[:, tt:tt + 1], in_=xs[tt][:], axis=AX.X,
                                                                     op=ALU.add)),
                              reads=[bxs[tt]], writes=[bss[tt]])
                    S_.op(ACT, lambda e: e.activation(out=st8[:, 4:8], in_=st8[:, 0:4], func=AF.Ln, bias=epsb[:],
                                                      scale=1.0 / D),
                          reads=bss + [bC], writes=[brs])
                    S_.op(ACT, lambda e: e.activation(out=st8[:, 4:8], in_=st8[:, 4:8], func=AF.Exp, scale=-0.5),
                          writes=[brs])
                    for tt in range(4):
                        S_.op(DVE, (lambda e, tt=tt: e.scalar_tensor_tensor(out=xs[tt][:], in0=xt[tt][:],
                                                                            scalar=st8[:, 4 + tt:5 + tt], in1=g1[:],
                                                                            op0=ALU.mult, op1=ALU.mult)),
                              reads=[bxt[tt], brs, bG], writes=[bxs[tt]])

                def stageA2(c, tt):
                    hb = c % 2
                    k4 = tt
                    pbf = pbank[0][:].bitcast(BF16)

                    def tr(e):
                        ins = None
                        for dc in range(8):
                            ins = e.transpose(pbf[:, dc * 128:(dc + 1) * 128], xs[k4][:, dc * 128:(dc + 1) * 128],
                                              ident[:])
                        return ins
                    S_.op(PE, tr, reads=[bxs[k4], bC], writes=[bP[0]])
                    S_.op(DVE, lambda e: e.tensor_copy(out=hnT[hb][:, :, tt * 128:(tt + 1) * 128],
                                                       in_=pbf.rearrange("p (c t) -> p c t", c=8)),
                          reads=[bP[0]], pwrites=[bhn[hb]])

                def proj(hb, col0, ncols, pidx, tcols=slice(0, 512)):
                    def f(e):
                        ins = None
                        for dc in range(8):
                            ins = e.matmul(pbank[pidx][0:ncols, :], lhsT=w_sb[:, dc, col0:col0 + ncols],
                                           rhs=hnT[hb][:, dc, :], start=(dc == 0), stop=(dc == 7))
                        return ins
                    S_.op(PE, f, reads=bWs + [bhn[hb]], writes=[bP[pidx]])

                cnt = {"qo": 0, "sq": 0}

                def stageB(c):
                    hb = c % 2
                    cs = slice(c * 512, (c + 1) * 512)
                    if c + 1 < NCH:
                        stageA1(c + 1)
                    PA = (1, 2, 5)
                    proj(hb, 0, 128, PA[0])
                    for j in range(16):
                        pa = PA[j % 3]
                        pm = 3 + (j % 2)
                        if c + 1 < NCH and j % 4 == 2:
                            stageA2(c + 1, j // 4)
                        if j + 1 < 16:
                            proj(hb, (j + 1) * 128, 128, PA[(j + 1) % 3])
                        si = cnt["sq"] % 2
                        cnt["sq"] += 1
                        S_.op(ACT, (lambda e, pa=pa, si=si: e.activation(out=sq[si][:], in_=pbank[pa][:],
                                                                         func=AF.Square)),
                              reads=[bP[pa]], writes=[bsq[si]])
                        S_.op(PE, (lambda e, pm=pm, si=si: e.matmul(pbank[pm][:], lhsT=blk[:], rhs=sq[si][:],
                                                                    start=True, stop=True)),
                              reads=[bsq[si], bC], writes=[bP[pm]])
                        S_.op(ACT, (lambda e, pm=pm, si=si: e.activation(out=lnt[si][:], in_=pbank[pm][:],
                                                                         func=AF.Ln, bias=epsb[:])),
                              reads=[bP[pm], bC], writes=[blnt[si]])
                        bias_t = qsb if j < 8 else zerob
                        S_.op(ACT, (lambda e, si=si, bias_t=bias_t: e.activation(out=lnt[si][:], in_=lnt[si][:],
                                                                                 func=AF.Exp, scale=-0.5,
                                                                                 bias=bias_t[:])),
                              reads=[bC], writes=[blnt[si]])
                        qi = cnt["qo"] % 3
                        cnt["qo"] += 1
                        S_.op(DVE, (lambda e, pa=pa, si=si, qi=qi, j=j: e.scalar_tensor_tensor(
                            out=qo[qi][:], in0=pbank[pa][:], scalar=gq[:, j:j + 1], in1=lnt[si][:],
                            op0=ALU.mult, op1=ALU.mult)),
                              reads=[bP[pa], blnt[si], bG], writes=[bqo[qi]])
                        dst, bdst = (QT, bQT) if j < 8 else (KT, bKT)
                        r0 = (j % 8) * 128
                        S_.dma(SP, (lambda e, qi=qi, dst=dst, r0=r0: e.dma_start(out=dst[r0:r0 + 128, cs],
                                                                                 in_=qo[qi][:])),
                               bqo[qi], store=True, reads=[bqo[qi]], pwrites=[bdst])
                    for i in range(3):
                        pa = 1 + (i % 2)
                        proj(hb, 3078 + i * 128, 128, pa)
                        si = cnt["sq"] % 2
                        cnt["sq"] += 1
                        S_.op(ACT, (lambda e, pa=pa, si=si: e.activation(out=lnt[si][:], in_=pbank[pa][:],
                                                                         func=AF.Exp, scale=-1.0)),
                              reads=[bP[pa]], writes=[blnt[si]])
                        S_.op(DVE, (lambda e, si=si: e.tensor_scalar(out=lnt[si][:], in0=lnt[si][:], scalar1=1.0,
                                                                     scalar2=None, op0=ALU.add)),
                              writes=[blnt[si]])
                        qi = cnt["qo"] % 3
                        cnt["qo"] += 1
                        S_.op(DVE, (lambda e, si=si, qi=qi: e.reciprocal(out=qo[qi][:], in_=lnt[si][:])),
                              reads=[blnt[si]], writes=[bqo[qi]])
                        S_.dma(SP, (lambda e, qi=qi, i=i: e.dma_start(out=GT[i * 128:(i + 1) * 128, cs],
                                                                      in_=qo[qi][:])),
                               bqo[qi], store=True, reads=[bqo[qi]], pwrites=[bGT])
                    proj(hb, 3072, N_FOX, 7)
                    S_.op(ACT, lambda e: e.activation(out=fr[:], in_=pbank[7][0:N_FOX, :], func=AF.Exp,
                                                      scale=-1.0, bias=negb[:]),
                          reads=[bP[7], bG], writes=[bfr])
                    S_.op(ACT, lambda e: e.activation(out=fr[:], in_=fr[:], func=AF.Ln, bias=oneb[0:N_FOX, :]),
                          reads=[bC], writes=[bfr])
                    ci = c % 2
                    init = 0.0 if c == 0 else cn[1 - ci][:, 511:512]
                    S_.op(DVE, (lambda e, ci=ci, init=init: e.tensor_tensor_scan(
                        out=cn[ci][:], data0=ones6[:], data1=fr[:], initial=init, op0=ALU.mult, op1=ALU.add)),
                          reads=[bfr, bG, bcn[1 - ci]], writes=[bcn[ci]])
                    S_.op(DVE, (lambda e, ci=ci: e.tensor_copy(out=pb3[ci][:, 0, :], in_=cn[ci][:])),
                          reads=[bcn[ci]], writes=[bp3[ci]])
                    S_.op(DVE, (lambda e, ci=ci: e.tensor_tensor(out=fr[:], in0=cn[ci][:], in1=pb3[ci][:, 0, :],
                                                                 op=ALU.subtract)),
                          reads=[bcn[ci], bp3[ci]], writes=[bfr])
                    S_.op(DVE, (lambda e, ci=ci: e.tensor_copy(out=pb3[ci][:, 1, :], in_=fr[:])),
                          reads=[bfr], pwrites=[bp3[ci]])
                    S_.op(DVE, (lambda e, ci=ci: e.tensor_tensor(out=fr2[:], in0=fr[:], in1=pb3[ci][:, 1, :],
                                                                 op=ALU.subtract)),
                          reads=[bfr, bp3[ci]], writes=[bfr2])
                    S_.op(DVE, (lambda e, ci=ci: e.tensor_copy(out=pb3[ci][:, 2, :], in_=fr2[:])),
                          reads=[bfr2], pwrites=[bp3[ci]])
                    S_.op(DVE, (lambda e, ci=ci: e.tensor_scalar(out=pa3[ci][:], in0=pb3[ci][:], scalar1=-1.0,
                                                                 scalar2=None, op0=ALU.mult)),
                          reads=[bp3[ci]], pwrites=[bp3[ci]])
                    S_.dma(SP, (lambda e, ci=ci: e.dma_start(out=CUMA[:, :, cs], in_=pa3[ci][:])),
                           bp3[ci], store=True, reads=[bp3[ci]], pwrites=[bCA])
                    S_.dma(SP, (lambda e, ci=ci: e.dma_start(out=CUMB[:, :, cs], in_=pb3[ci][:])),
                           bp3[ci], store=True, reads=[bp3[ci]], pwrites=[bCB])
                    for tt in range(4):
                        vi = (c * 4 + tt) % 2
                        t0 = c * 512 + tt * 128
                        for half in range(2):
                            pv = 5 + half

                            def f(e, pv=pv, tt=tt, half=half):
                                ins = None
                                for dc in range(8):
                                    ins = e.matmul(pbank[pv][:], lhsT=hnT[hb][:, dc, tt * 128:(tt + 1) * 128],
                                                   rhs=w_sb[:, dc, 2048 + half * 512:2048 + (half + 1) * 512],
                                                   start=(dc == 0), stop=(dc == 7))
                                return ins
                            S_.op(PE, f, reads=bWs + [bhn[hb]], writes=[bP[pv]])
                            S_.op(ACT, (lambda e, pv=pv, vi=vi, half=half: e.copy(
                                out=vst[vi][:, half * 8:(half + 1) * 8, 0:64],
                                in_=pbank[pv][:].rearrange("p (h d) -> p h d", h=8))),
                                  reads=[bP[pv]], pwrites=[bvst[vi]])
                        S_.dma(SP, (lambda e, vi=vi, t0=t0: e.dma_start(out=V[t0:t0 + 128, :, :], in_=vst[vi][:])),
                               bvst[vi], store=True, reads=[bvst[vi]], pwrites=[bV])

                loadX(0)
                stageA1(0)
                for tt in range(4):
                    stageA2(0, tt)
                if NCH > 1:
                    loadX(1)
                for c in range(NCH):
                    stageB(c)
                    if c + 2 < NCH:
                        loadX(c + 2)
            S_.barrier()

        def phase2(l):
            with ExitStack() as es:
                ogc = sbt(es, "ogc", [64, 16], F32)
                negm = sbt(es, "negm", [128, NT * 16], F32)
                past = sbt(es, "past", [128, NT * 16], F32)
                own = sbt(es, "own", [128, NT * 16], F32)
                bK = Buf("p2consts")
                S_.dma(SP, lambda e: e.dma_start(out=ogc[:], in_=og_col[l]), bK, pwrites=[bK])
                for nm, t_ in (("negm", negm), ("past", past), ("own", own)):
                    S_.dma(SP, (lambda e, t_=t_, nm=nm: e.dma_start(out=t_[:], in_=cd[nm])), bK, pwrites=[bK])
                RMAX = 86
                qa = [sbt(es, "qa%d" % i, [RMAX, S], BF16) for i in range(2)]
                ka = [sbt(es, "ka%d" % i, [RMAX, S], BF16) for i in range(2)]
                bqa = [Buf("qa%d" % i) for i in range(2)]
                bka = [Buf("ka%d" % i) for i in range(2)]
                va = [sbt(es, "va%d" % i, [128, NT, 65], BF16) for i in range(2)]
                bva = [Buf("va%d" % i) for i in range(2)]
                vd = [[sbt(es, "vd%d_%d" % (j, i), [128, NT, 65], BF16) for i in range(2)] for j in range(2)]
                bvd = [[[Buf("vd%d_%d_%d" % (j, i, r)) for r in range(dd)] for i, dd in enumerate((4, 16))]
                       for j in range(2)]
                pT = [sbt(es, "pT%d" % i, [128, 512], BF16) for i in range(4)]
                bpT = [Buf("pT%d" % i) for i in range(4)]
                sq65 = sbt(es, "sq65", [65, 512], BF16)
                bsq65 = Buf("sq65")
                rden = sbt(es, "rden", [65, 512], F32)
                brden = Buf("rden")
                t0 = sbt(es, "t0", [64, 512], F32)
                bt0 = Buf("t0")
                S_.op(POOL, lambda e: e.memset(SSQ[:], 0.0), writes=[bSSQ])
                lnf = sbt(es, "lnf", [64, 512], F32)
                blnf = Buf("lnf")
                yf = [sbt(es, "yf%d" % i, [64, 512], BF16) for i in range(2)]
                byf = [Buf("yf%d" % i) for i in range(2)]
                gt = [sbt(es, "gt%d" % i, [64, 512], BF16) for i in range(2)]
                bgt = [Buf("gt%d" % i) for i in range(2)]
                kms = sbt(es, "kms", [64, 16], F32)
                kmb = sbt(es, "kmb", [64, 16], BF16)
                bkm = Buf("km")
                gm2 = [sbt(es, "gm%d" % i, [128, 16], F32) for i in range(3)]
                top82 = [sbt(es, "top8%d" % i, [128, 8], F32) for i in range(3)]
                t12 = [sbt(es, "t1%d" % i, [128, 16], F32) for i in range(3)]
                t22 = [sbt(es, "t2%d" % i, [128, 16], F32) for i in range(3)]
                bgm2, btop82, bt12, bt22 = [[Buf("%s%d" % (n, i)) for i in range(3)] for n in ("gm", "top8", "t1", "t2")]
                zp = [sbt(es, "zp%d" % i, [128, 80], BF16) for i in range(3)]
                bzp = [Buf("zp%d" % i) for i in range(3)]
                zero1 = sbt(es, "zero1", [1, 128], BF16)
                for i in range(3):
                    S_.op(POOL, (lambda e, i=i: e.memset(zp[i][:], 0.0)), writes=[bzp[i]])
                S_.op(POOL, lambda e: e.memset(zero1[:], 0.0), pwrites=[bK])
                ycnt = [0]

                def finalize(h, pob, c, gated_gi=None):
                    cs = slice(c * 512, (c + 1) * 512)
                    g = 0 if h < 4 else (1 if h < 10 else 2)
                    def partA():
                        S_.op(ACT, lambda e: e.activation(out=rden[64:65, :], in_=pbank[pob][64:65, :], func=AF.Ln),
                              reads=[bP[pob]], writes=[brden])

                    def partB():
                        S_.op(PE, lambda e: e.matmul(pbank[7][0:64, :], lhsT=onesf[64:65, :], rhs=rden[64:65, :],
                                                     start=True, stop=True),
                              reads=[brden, bC], writes=[bP[7]])
                        S_.op(ACT, lambda e: e.activation(out=lnf[:], in_=pbank[7][0:64, :], func=AF.Exp, scale=-1.0),
                              reads=[bP[7]], writes=[blnf])
                        S_.op(DVE, lambda e: e.tensor_tensor(out=t0[:], in0=pbank[pob][0:64, :], in1=lnf[:],
                                                             op=ALU.mult),
                              reads=[bP[pob], blnf], writes=[bt0])
                        S_.op(ACT, lambda e: e.activation(out=sq65[0:64, :], in_=t0[:], func=AF.Square),
                              reads=[bt0], writes=[bsq65])
                        yi = ycnt[0] % 2
                        ycnt[0] += 1
                        S_.op(DVE, lambda e: e.tensor_scalar(out=yf[yi][:], in0=t0[:], scalar1=ogc[:, h:h + 1],
                                                             scalar2=None, op0=ALU.mult),
                              reads=[bt0, bK], writes=[byf[yi]])
                        if gated_gi is not None:
                            S_.op(POOL, lambda e: e.tensor_tensor(out=yf[yi][:], in0=yf[yi][:],
                                                                  in1=gt[gated_gi][:], op=ALU.mult),
                                  reads=[bgt[gated_gi]], writes=[byf[yi]])
                        S_.dma(SP, lambda e: e.dma_start(out=YT[h * 64:(h + 1) * 64, cs], in_=yf[yi][:]),
                               byf[yi], store=True, reads=[byf[yi]], pwrites=[bYT])

                    def partC():
                        def fss(e):
                            ins = None
                            for i in range(4):
                                ins = e.matmul(pbank[7][:, i:i + 1], lhsT=sq65[0:64, i * 128:(i + 1) * 128],
                                               rhs=c65[:], start=True, stop=True)
                            return ins
                        S_.op(PE, fss, reads=[bsq65, bC], writes=[bP[7]])
                        S_.op(DVE, lambda e: e.tensor_tensor(out=SSQ[:, 4 * c:4 * c + 4, g],
                                                             in0=SSQ[:, 4 * c:4 * c + 4, g],
                                                             in1=pbank[7][:, 0:4], op=ALU.add),
                              reads=[bP[7]], pwrites=[bSSQ])
                    deferred.append([1, partA])
                    deferred.append([3, partB])
                    deferred.append([6, partC])

                def load_head(h, hi, kind):
                    S_.dma(SP, lambda e: e.dma_start(out=qa[hi][0:64, :], in_=QT[h * 64:(h + 1) * 64, :]),
                           bqa[hi], reads=[bQT], writes=[bqa[hi]])
                    S_.dma(SP, lambda e: e.dma_start(out=ka[hi][0:64, :], in_=KT[h * 64:(h + 1) * 64, :]),
                           bka[hi], reads=[bKT], writes=[bka[hi]])
                    if kind == "fox":
                        hf = h - 4
                        r = 64
                        S_.dma(SP, lambda e: e.dma_start(out=qa[hi][r:r + 3, :], in_=CUMA[hf]), bqa[hi],
                               reads=[bCA], pwrites=[bqa[hi]])
                        S_.dma(SP, lambda e: e.dma_start(out=ka[hi][r + 3:r + 6, :], in_=CUMB[hf]), bka[hi],
                               reads=[bCB], pwrites=[bka[hi]])
                    else:
                        r = 80 if kind == "moba" else 64
                        S_.dma(SP, lambda e: e.dma_start(out=qa[hi][r:r + 3, :], in_=cd["posA"][h]), bqa[hi],
                               pwrites=[bqa[hi]])
                        S_.dma(SP, lambda e: e.dma_start(out=ka[hi][r + 3:r + 6, :], in_=cd["posB"][h]), bka[hi],
                               pwrites=[bka[hi]])
                    S_.dma(SP, lambda e: e.dma_start(out=qa[hi][r + 3:r + 6, :], in_=cd["ones3"]), bqa[hi],
                           pwrites=[bqa[hi]])
                    S_.dma(SP, lambda e: e.dma_start(out=ka[hi][r:r + 3, :], in_=cd["ones3"]), bka[hi],
                           pwrites=[bka[hi]])
                    if kind == "moba":
                        S_.dma(SP, lambda e: e.dma_start(out=ka[hi][64:80, :], in_=cd["ind16"]), bka[hi],
                               pwrites=[bka[hi]])

                def load_v(h, vt, bvts_, d):
                    for r in range(d):
                        src = V.rearrange("(b p r) h e -> p b r h e", p=128, r=d)[:, :, r, h, :]
                        dst = vt[:].rearrange("p (b r) e -> p b r e", r=d)[:, :, r, :]
                        S_.dma(SP, (lambda e, src=src, dst=dst: e.dma_start(out=dst, in_=src)), bvts_[r], reads=[bV],
                               writes=[bvts_[r]])

                def moba_prelude_items(h, hi):
                    def head_part():
                        S_.op(DVE, lambda e: e.memset(kms[:], 0.0), writes=[bkm])
                        S_.op(DVE, lambda e: e.tensor_reduce(out=kms[:, 0:S // 256],
                                                             in_=ka[hi][0:64, :].rearrange("p (n k) -> p n k", k=256),
                                                             axis=AX.X, op=ALU.add),
                              reads=[bka[hi]], writes=[bkm])
                        S_.op(DVE, lambda e: e.tensor_copy(out=kmb[:], in_=kms[:]), writes=[bkm])

                    def p1(i):
                        zi = i % 3
                        sl = slice(i * 16, (i + 1) * 16)
                        gc = slice((i % 8) * 16, (i % 8) * 16 + 16)
                        gm, top8, t1, t2 = gm2[zi], top82[zi], t12[zi], t22[zi]
                        bgm, btop8, bt1, bt2 = bgm2[zi], btop82[zi], bt12[zi], bt22[zi]
                        S_.op(PE, lambda e: e.matmul(pbank[6][:, gc], lhsT=qa[hi][0:64, i * 128:(i + 1) * 128],
                                                     rhs=kmb[:], start=True, stop=True),
                              reads=[bqa[hi], bkm], writes=[bP[6]])
                        S_.op(DVE, lambda e: e.tensor_tensor(out=gm[:], in0=pbank[6][:, gc], in1=negm[:, sl],
                                                             op=ALU.add),
                              reads=[bP[6], bK], writes=[bgm])
                        S_.op(DVE, lambda e: e.max(out=top8[:], in_=gm[:]), reads=[bgm], writes=[btop8])
                        S_.op(DVE, lambda e: e.scalar_tensor_tensor(out=t1[:], in0=gm[:], scalar=top8[:, 2:3],
                                                                    in1=past[:, sl], op0=ALU.is_ge, op1=ALU.mult),
                              reads=[bgm, btop8, bK], writes=[bt1])
                        S_.op(DVE, lambda e: e.tensor_tensor(out=t2[:], in0=t1[:], in1=own[:, sl], op=ALU.add),
                              reads=[bt1, bK], writes=[bt2])
                        S_.op(DVE, lambda e: e.tensor_scalar(out=zp[zi][:, 64:80], in0=t2[:], scalar1=-1.0,
                                                             scalar2=-MASKNEG, op0=ALU.add, op1=ALU.mult),
                              reads=[bt2], pwrites=[bzp[zi]])

                    def p2(i):
                        zi = i % 3
                        mc = slice((i % 2) * 128, (i % 2) * 128 + 128)
                        S_.op(PE, lambda e: e.matmul(pbank[7][0:80, mc], lhsT=zp[zi][:, 0:80], rhs=ident[:],
                                                     start=True, stop=True),
                              reads=[bzp[zi], bC], writes=[bP[7]])
                        S_.op(ACT, lambda e: e.copy(out=qa[hi][64:80, i * 128:(i + 1) * 128], in_=pbank[7][64:80, mc]),
                              reads=[bP[7]], pwrites=[bqa[hi]])

                    def item(k):
                        def f():
                            if k - 2 >= 0:
                                p2(k - 2)
                            if k < NT:
                                p1(k)
                        return f
                    return [head_part] + [item(k) for k in range(NT + 2)]

                LOOK = 2
                deferred = []

                def tick():
                    keep = []
                    for item in deferred:
                        item[0] -= 1
                        if item[0] <= 0:
                            item[1]()
                        else:
                            keep.append(item)
                    deferred[:] = keep

                def flush():
                    while deferred:
                        tick()

                bg = []

                def run_pipeline(steps, LOOK=2):
                    n = len(steps)
                    for i in range(min(LOOK, n)):
                        steps[i][0]()
                    for k in range(n):
                        if k + LOOK < n:
                            steps[k + LOOK][0]()
                        steps[k][1]()
                        tick()
                        if bg and k % 4 == 1:
                            bg.pop(0)()
                    while bg:
                        bg.pop(0)()

                def causal_head(h, hi, kind):
                    R = 86 if kind == "moba" else 70
                    if debug and h == 0:
                        S_.dma(SP, lambda e: e.dma_start(out=DBGQ, in_=qa[hi][0:86, :]), bDBG, store=True,
                               reads=[bqa[hi]], pwrites=[bDBG])
                    steps = []
                    k = 0
                    for c in range(NCH):
                        for jt in range(4 * c + 4):
                            steps.append(mk_causal_step(h, hi, kind, R, c, jt, k))
                            k += 1
                    run_pipeline(steps, LOOK=3)

                def mk_causal_step(h, hi, kind, R, c, jt, k):
                    njt = 4 * c + 4
                    m = jt - 4 * c
                    off = max(m, 0) * 128
                    ps = k % 4
                    po = 4 + (c % 2)

                    def qk():
                        S_.op(PE, lambda e: e.matmul(pbank[ps][:, off:512], lhsT=ka[hi][0:R, jt * 128:(jt + 1) * 128],
                                                     rhs=qa[hi][0:R, c * 512 + off:(c + 1) * 512],
                                                     start=True, stop=True),
                              reads=[bqa[hi], bka[hi]], writes=[bP[ps]])

                    def rest():
                        if jt == 0 and kind == "fox":
                            gi = c % 2
                            S_.dma(SP, lambda e: e.dma_start(
                                out=gt[gi][:], in_=GT[(h - 4) * 64:(h - 3) * 64, c * 512:(c + 1) * 512]),
                                   bgt[gi], reads=[bGT], writes=[bgt[gi]])
                        if m >= 0:
                            S_.op(DVE, lambda e: e.tensor_tensor(
                                out=pbank[ps][:, off:off + 128], in0=pbank[ps][:, off:off + 128], in1=tri[:],
                                op=ALU.add), reads=[bC], writes=[bP[ps]])
                        S_.op(ACT, lambda e: e.activation(out=pT[ps][:, off:512], in_=pbank[ps][:, off:512],
                                                          func=AF.Exp),
                              reads=[bP[ps]], writes=[bpT[ps]])
                        S_.op(PE, lambda e: e.matmul(pbank[po][0:65, off:512], lhsT=va[hi][:, jt, :],
                                                     rhs=pT[ps][:, off:512], start=(jt == 0), stop=(jt == njt - 1)),
                              reads=[bpT[ps], bva[hi]], writes=[bP[po]] if jt == 0 else (),
                              pwrites=() if jt == 0 else [bP[po]])
                        if jt == njt - 1:
                            finalize(h, po, c, gated_gi=(c % 2) if kind == "fox" else None)
                    return (qk, rest)

                def dil_head(h, hi):
                    R = 70
                    dils = (1, 4, 16)
                    vts = (va[hi], vd[hi][0], vd[hi][1])
                    bvts = ([bva[hi]], bvd[hi][0], bvd[hi][1])
                    SC = min(2048, S)
                    nq = SC // 512
                    kk = [0]
                    for sc in range(S // SC):
                        flush()
                        for q in range(nq):
                            S_.op(PE, (lambda e, q=q: e.matmul(pbank[3 + q][0:65, :], lhsT=zero1[0:1, 0:65],
                                                               rhs=qa[hi][0:1, 0:512], start=True, stop=False)),
                                  reads=[bK, bqa[hi]], writes=[bP[3 + q]])
                        blocks = []
                        for di, d in enumerate(dils):
                            nblk = SC // (d * 128)
                            for r in range(d):
                                for b in range(sc * nblk, (sc + 1) * nblk):
                                    blocks.append((di, d, r, b))
                        steps = []
                        for i in range(0, len(blocks), 2):
                            steps.append(mk_dil_step(h, hi, R, sc, SC, nq, blocks[i:i + 2], kk[0], vts, bvts))
                            kk[0] += 1
                        run_pipeline(steps)
                        for q in range(nq):
                            S_.op(PE, (lambda e, q=q: e.matmul(pbank[3 + q][0:65, :], lhsT=zero1[0:1, 0:65],
                                                               rhs=qa[hi][0:1, 0:512], start=False, stop=True)),
                                  reads=[bK, bqa[hi]], pwrites=[bP[3 + q]])
                        for q in range(nq):
                            finalize(h, 3 + q, (sc * SC) // 512 + q)
                            flush()

                def mk_dil_step(h, hi, R, sc, SC, nq, blks, k, vts, bvts):
                    ps = k % 3
                    W = 256 * len(blks)

                    def qk():
                        def f(e):
                            ins = None
                            for i, (di, d, r, b) in enumerate(blks):
                                qv = qa[hi][0:R, :].rearrange("p (i d) -> p d i", d=d)
                                kv = ka[hi][0:R, :].rearrange("p (i d) -> p d i", d=d)
                                for s_, kb in ((0, b - 1), (1, b)):
                                    if kb < 0:
                                        ins = e.matmul(pbank[ps][:, i * 256:i * 256 + 128], lhsT=zero1[0:1, :],
                                                       rhs=qv[0:1, r, b * 128:(b + 1) * 128], start=True, stop=True)
                                        continue
                                    ins = e.matmul(pbank[ps][:, i * 256 + s_ * 128:i * 256 + (s_ + 1) * 128],
                                                   lhsT=kv[:, r, kb * 128:(kb + 1) * 128],
                                                   rhs=qv[:, r, b * 128:(b + 1) * 128], start=True, stop=True)
                            return ins
                        S_.op(PE, f, reads=[bqa[hi], bka[hi], bK], writes=[bP[ps]])

                    def rest():
                        S_.op(DVE, lambda e: e.tensor_tensor(out=pbank[ps][:, 0:W], in0=pbank[ps][:, 0:W],
                                                             in1=band[:, 0:W], op=ALU.add),
                              reads=[bC], writes=[bP[ps]])
                        S_.op(ACT, lambda e: e.activation(out=pT[ps][:, 0:W], in_=pbank[ps][:, 0:W], func=AF.Exp),
                              reads=[bP[ps]], writes=[bpT[ps]])
                        banks = set()
                        plan = []
                        for i, (di, d, r, b) in enumerate(blks):
                            tstart = r + d * b * 128 - sc * SC
                            span = d * 128
                            segs = []
                            if span <= 512:
                                q = tstart // 512
                                base = tstart - q * 512
                                if d == 1:
                                    oview = pbank[3 + q][0:65, base:base + 128]
                                else:
                                    oview = pbank[3 + q][0:65, :].rearrange("p (u d) -> p d u", d=d)[:, r, :]
                                segs.append((oview, 0, 128))
                                banks.add(3 + q)
                            else:
                                per = 512 // d
                                for q in range(nq):
                                    oview = pbank[3 + q][0:65, :].rearrange("p (u d) -> p d u", d=d)[:, r, :]
                                    segs.append((oview, q * per, per))
                                    banks.add(3 + q)
                            slots = [(1, b)] if b == 0 else [(0, b - 1), (1, b)]
                            plan.append((i, di, d, r, slots, segs))

                        def fpv(e):
                            ins = None
                            for (i, di, d, r, slots, segs) in plan:
                                for (s_, kb) in slots:
                                    for (oview, u0, n) in segs:
                                        c0 = i * 256 + s_ * 128 + u0
                                        ins = e.matmul(oview, lhsT=vts[di][:, kb * d + r, :], rhs=pT[ps][:, c0:c0 + n],
                                                       start=False, stop=False)
                            return ins
                        S_.op(PE, fpv, reads=[bpT[ps]] + [bvts[p[1]][p[3] if p[1] > 0 else 0] for p in plan],
                              pwrites=[bP[x] for x in sorted(banks)])
                    return (qk, rest)

                heads = [(h, "moba") for h in range(0, 4)] + [(h, "fox") for h in range(4, 10)] + \
                        [(h, "dil") for h in range(10, 16)]
                def load_all(idx):
                    h, kind = heads[idx]
                    hi = idx % 2
                    load_head(h, hi, kind)
                    load_v(h, va[hi], [bva[hi]], 1)
                    if kind == "dil":
                        load_v(h, vd[hi][0], bvd[hi][0], 4)
                        load_v(h, vd[hi][1], bvd[hi][1], 16)

                load_all(0)
                for idx, (h, kind) in enumerate(heads):
                    hi = idx % 2
                    if idx + 1 < len(heads):
                        load_all(idx + 1)
                    if kind == "moba" and idx == 0:
                        for it in moba_prelude_items(h, hi):
                            it()
                    if idx + 1 < len(heads) and heads[idx + 1][1] == "moba":
                        bg.extend(moba_prelude_items(heads[idx + 1][0], (idx + 1) % 2))
                    if kind == "dil":
                        dil_head(h, hi)
                    else:
                        causal_head(h, hi, kind)
                flush()
                for g, W in ((0, 256), (1, 384), (2, 384)):
                    S_.op(ACT, (lambda e, g=g, W=W: e.activation(out=RG[:, :, g], in_=SSQ[:, :, g], func=AF.Ln,
                                                                 bias=epsb[:], scale=1.0 / W)),
                          reads=[bSSQ, bC], pwrites=[bRG])
                    S_.op(ACT, (lambda e, g=g: e.activation(out=RG[:, :, g], in_=RG[:, :, g], func=AF.Exp,
                                                            scale=-0.5)), pwrites=[bRG])
            S_.barrier()

        def load_w3a(l, es):
            wo = sbt(es, "wo", [128, 8, D], BF16)
            wu = sbt(es, "wu", [128, 8, DFF], BF16)
            bwo = [Buf("wo%d" % i) for i in range(2)]
            bwu = [Buf("wu%d" % i) for i in range(4)]
            for i in range(2):
                S_.dma(POOL, (lambda e, i=i: e.dma_start(
                    out=wo[:, 4 * i:4 * i + 4, :],
                    in_=w_out[l, 512 * i:512 * (i + 1), :].rearrange("(k p) n -> p k n", p=128))),
                       bwo[i], writes=[bwo[i]])
            for i in range(4):
                S_.dma(POOL, (lambda e, i=i: e.dma_start(
                    out=wu[:, 2 * i:2 * i + 2, :],
                    in_=w_up[l, 256 * i:256 * (i + 1), :].rearrange("(k p) n -> p k n", p=128))),
                       bwu[i], writes=[bwu[i]])
            return wo, wu, bwo, bwu

        def phase3(l, x_src, bx_src, x_dst, bx_dst, w3a):
            TC = 256
            wo, wu, bwo, bwu = w3a
            with ExitStack() as es:
                wd = sbt(es, "wd", [128, 32, D], BF16)
                bwd = [Buf("wd%d" % i) for i in range(4)]
                for i in range(4):
                    S_.dma(POOL, (lambda e, i=i: e.dma_start(
                        out=wd[:, 8 * i:8 * i + 8, :],
                        in_=w_down[l, 1024 * i:1024 * (i + 1), :].rearrange("(k p) n -> p k n", p=128))),
                           bwd[i], writes=[bwd[i]])
                g2 = sbt(es, "g2", [128, D], F32)
                bG = Buf("p3consts")
                S_.dma(SP, lambda e: e.dma_start(out=g2[:], in_=mlp_norm[l:l + 1, :].partition_broadcast(128)),
                       bG, pwrites=[bG])
                yT = [sbt(es, "yT%d" % i, [128, 8, TC], BF16) for i in range(2)]
                byT = [Buf("yT%d" % i) for i in range(2)]
                xt = [sbt(es, "x3_%d" % i, [128, D], F32) for i in range(4)]
                bxt = [Buf("x3_%d" % i) for i in range(4)]
                st8 = sbt(es, "st83", [128, 8], F32)
                bss = [Buf("ss3_%d" % i) for i in range(2)]
                brs = Buf("rs3")
                xs = [sbt(es, "xs3_%d" % i, [128, D], BF16) for i in range(2)]
                bxs = [Buf("xs3_%d" % i) for i in range(2)]
                hnT = [sbt(es, "hn2T%d" % i, [128, 8, TC], BF16) for i in range(2)]
                bhn = [Buf("hn2T%d" % i) for i in range(2)]
                hT = sbt(es, "hT", [128, 32, TC], BF16)
                bhT = Buf("hT")
                rl = [sbt(es, "rl%d" % i, [128, 2 * TC], BF16) for i in range(2)]
                brl = [Buf("rl%d" % i) for i in range(2)]
                NC3 = S // TC
                NTT = TC // 128

                def loadY(c):
                    cb = c % 2
                    S_.dma(SP, (lambda e: e.dma_start(out=yT[cb][:], in_=YT.rearrange("(k p) s -> p k s", p=128)[:, :, c * TC:(c + 1) * TC])),
                           byT[cb], reads=[bYT], writes=[byT[cb]])

                def stageA1(c):
                    cb = c % 2
                    for tt in range(NTT):
                        k4 = (c * NTT + tt) % 4
                        t0 = c * TC + tt * 128
                        S_.dma(SP, (lambda e, k4=k4, t0=t0: e.dma_start(out=xt[k4][:], in_=x_src[t0:t0 + 128, :])),
                               bxt[k4], reads=[bx_src], writes=[bxt[k4]])
                    for tt in range(NTT):
                        k4 = (c * NTT + tt) % 4
                        k = tt % 2
                        t0 = c * TC + tt * 128
                        ti = t0 // 128
                        for half in range(2):
                            for g, kcs in ((0, (0, 1)), (1, (2, 3, 4)), (2, (5, 6, 7))):
                                pb = (half * 3 + g) % 2

                                def f(e, half=half, tt=tt, kcs=kcs, pb=pb):
                                    ins = None
                                    for kc in kcs:
                                        ins = e.matmul(pbank[pb][:], lhsT=yT[cb][:, kc, tt * 128:(tt + 1) * 128],
                                                       rhs=wo[:, kc, half * 512:(half + 1) * 512],
                                                       start=(kc == kcs[0]), stop=(kc == kcs[-1]))
                                    return ins
                                S_.op(PE, f, reads=[byT[cb]] + bwo, writes=[bP[pb]])
                                S_.op(DVE, (lambda e, half=half, k4=k4, pb=pb, g=g, ti=ti: e.scalar_tensor_tensor(
                                    out=xt[k4][:, half * 512:(half + 1) * 512], in0=pbank[pb][:],
                                    scalar=RG[:, ti, g:g + 1], in1=xt[k4][:, half * 512:(half + 1) * 512],
                                    op0=ALU.mult, op1=ALU.add)),
                                      reads=[bP[pb], bRG], writes=[bxt[k4]])
                        if debug and XM is not None:
                            S_.dma(SP, (lambda e, k4=k4, t0=t0: e.dma_start(out=XM[t0:t0 + 128, :], in_=xt[k4][:])),
                                   bxt[k4], store=True, reads=[bxt[k4]], pwrites=[bXM])
                    for tt in range(NTT):
                        k4 = (c * NTT + tt) % 4
                        S_.op(ACT, (lambda e, tt=tt, k4=k4: e.activation(out=xs[tt][:], in_=xt[k4][:], func=AF.Square)),
                              reads=[bxt[k4]], writes=[bxs[tt]])
                    for tt in range(NTT):
                        S_.op(DVE, (lambda e, tt=tt: e.tensor_reduce(out=st8[:, tt:tt + 1], in_=xs[tt][:], axis=AX.X,
                                                                     op=ALU.add)),
                              reads=[bxs[tt]], writes=[bss[tt]])
                    S_.op(ACT, lambda e: e.activation(out=st8[:, 4:4 + NTT], in_=st8[:, 0:NTT], func=AF.Ln, bias=epsb[:],
                                                      scale=1.0 / D),
                          reads=bss + [bC], writes=[brs])
                    S_.op(ACT, lambda e: e.activation(out=st8[:, 4:4 + NTT], in_=st8[:, 4:4 + NTT], func=AF.Exp,
                                                      scale=-0.5), writes=[brs])
                    for tt in range(NTT):
                        k4 = (c * NTT + tt) % 4
                        S_.op(DVE, (lambda e, tt=tt, k4=k4: e.scalar_tensor_tensor(out=xs[tt][:], in0=xt[k4][:],
                                                                                   scalar=st8[:, 4 + tt:5 + tt],
                                                                                   in1=g2[:], op0=ALU.mult,
                                                                                   op1=ALU.mult)),
                              reads=[bxt[k4], brs, bG], writes=[bxs[tt]])

                def stageA2(c):
                    cb = c % 2
                    for tt in range(NTT):
                        k = tt % 2
                        pbf = pbank[2][:].bitcast(BF16)

                        def tr(e, k=k, pbf=pbf):
                            ins = None
                            for dc in range(8):
                                ins = e.transpose(pbf[:, dc * 128:(dc + 1) * 128], xs[k][:, dc * 128:(dc + 1) * 128],
                                                  ident[:])
                            return ins
                        S_.op(PE, tr, reads=[bxs[k], bC], writes=[bP[2]])
                        S_.op(ACT, (lambda e, tt=tt, pbf=pbf: e.copy(
                            out=hnT[cb][:, :, tt * 128:(tt + 1) * 128],
                            in_=pbf.rearrange("p (c t) -> p c t", c=8))),
                              reads=[bP[2]], pwrites=[bhn[cb]])

                def stageBup(c):
                    cb = c % 2
                    for f2 in range(16):
                        pu = 2 + (f2 % 2)
                        ri = f2 % 2

                        def f(e, f2=f2, pu=pu):
                            ins = None
                            for s in range(2):
                                fc = f2 * 2 + s
                                for dc in range(8):
                                    ins = e.matmul(pbank[pu][:, s * TC:(s + 1) * TC],
                                                   lhsT=wu[:, dc, fc * 128:(fc + 1) * 128], rhs=hnT[cb][:, dc, :],
                                                   start=(dc == 0), stop=(dc == 7))
                            return ins
                        S_.op(PE, f, reads=bwu + [bhn[cb]], writes=[bP[pu]])
                        S_.op(ACT, (lambda e, pu=pu, ri=ri: e.activation(out=rl[ri][:], in_=pbank[pu][:],
                                                                         func=AF.Relu)),
                              reads=[bP[pu]], writes=[brl[ri]])
                        eng = POOL if f2 % 2 else DVE
                        S_.op(eng, (lambda e, f2=f2, ri=ri: e.tensor_tensor(
                            out=hT[:, 2 * f2:2 * f2 + 2, :], in0=rl[ri][:].rearrange("p (s t) -> p s t", s=2),
                            in1=rl[ri][:].rearrange("p (s t) -> p s t", s=2), op=ALU.mult)),
                              reads=[brl[ri]], pwrites=[bhT])

                def stageBdown(c):
                    def f(e):
                        ins = None
                        for fc in range(32):
                            for tt in range(NTT):
                                for half in range(2):
                                    ins = e.matmul(pbank[4 + tt * 2 + half][:],
                                                   lhsT=hT[:, fc, tt * 128:(tt + 1) * 128],
                                                   rhs=wd[:, fc, half * 512:(half + 1) * 512],
                                                   start=(fc == 0), stop=(fc == 31))
                        return ins
                    S_.op(PE, f, reads=[bhT] + bwd, writes=[bP[4 + i] for i in range(2 * NTT)])
                    for tt in range(NTT):
                        k4 = (c * NTT + tt) % 4
                        t0 = c * TC + tt * 128
                        for half in range(2):
                            S_.op(DVE, (lambda e, tt=tt, half=half, k4=k4: e.tensor_tensor(
                                out=xt[k4][:, half * 512:(half + 1) * 512], in0=pbank[4 + tt * 2 + half][:],
                                in1=xt[k4][:, half * 512:(half + 1) * 512], op=ALU.add)),
                                  reads=[bP[4 + tt * 2 + half]], writes=[bxt[k4]])
                        S_.dma(SP, (lambda e, k4=k4, t0=t0: e.dma_start(out=x_dst[t0:t0 + 128, :], in_=xt[k4][:])),
                               bxt[k4], store=True, reads=[bxt[k4]], pwrites=[bx_dst])

                loadY(0)
                loadY(1)
                stageA1(0)
                stageA2(0)
                for c in range(NC3):
                    if c + 1 < NC3:
                        stageA1(c + 1)
                    if c + 2 < NC3:
                        loadY(c + 2)
                    stageBup(c)
                    if c + 1 < NC3:
                        stageA2(c + 1)
                    stageBdown(c)
            S_.barrier()

        bxin = Buf("x_in")
        for l in range(NL):
            x_src, bsrc = (x_in, bxin) if l == 0 else (X1, bX1)
            x_dst, bdst = (out, bOUT) if l == NL - 1 else (X1, bX1)
            phase1(l, x_src, bsrc)
            with ExitStack() as wes:
                w3a = load_w3a(l, wes)
                phase2(l)
                phase3(l, x_src, bsrc, x_dst, bdst, w3a)
        S_.emit(final_waits=_compress(bOUT.writers))
        S_.close()
    return nc, consts_np


def _prep_weights(inp, NL):
    f = lambda a: np.ascontiguousarray(np.asarray(a, dtype=np.float32))
    qg = f(inp["q_gain"])
    kg = f(inp["k_gain"])
    og = f(inp["out_gain"])
    m = {
        "attn_norm": f(inp["attn_norm"])[:NL],
        "w_in": f(inp["w_in"])[:NL],
        "b_forget": f(inp["b_forget"])[:NL].reshape(NL, N_FOX, 1),
        "qg_col": np.ascontiguousarray(qg[:NL].reshape(NL, 8, 2, 64).transpose(0, 2, 3, 1).reshape(NL, 128, 8)),
        "kg_col": np.ascontiguousarray(kg[:NL].reshape(NL, 8, 2, 64).transpose(0, 2, 3, 1).reshape(NL, 128, 8)),
        "og_col": np.ascontiguousarray(og[:NL].reshape(NL, 16, 64).transpose(0, 2, 1)),
        "w_out": f(inp["w_out"])[:NL],
        "mlp_norm": f(inp["mlp_norm"])[:NL],
        "w_up": f(inp["w_up"])[:NL],
        "w_down": f(inp["w_down"])[:NL],
    }
    return m


_CACHE = {}


def kernel(**inputs):
    x = np.asarray(inputs["x"], dtype=np.float32)
    B, S, _ = x.shape
    NL = 2
    key = (S, NL)
    if key not in _CACHE:
        _CACHE[key] = build(S, NL)
    nc, consts = _CACHE[key]
    wm = _prep_weights(inputs, NL)
    for k, v in consts.items():
        wm["c_" + k] = v
    in_maps = []
    for b in range(B):
        m = dict(wm)
        m["x"] = np.ascontiguousarray(x[b])
        in_maps.append(m)
    res = run_bass_kernel_spmd(nc, in_maps, core_ids=list(range(B)))
    return np.stack([np.asarray(r["out"], dtype=np.float32) for r in res.results], axis=0)
```

```python
from contextlib import ExitStack
import numpy as np
import ml_dtypes
import concourse.bass as bass
import concourse.mybir as mybir
from concourse.bass_utils import run_bass_kernel_spmd

F32 = mybir.dt.float32
BF16 = mybir.dt.bfloat16
ALU = mybir.AluOpType
AF = mybir.ActivationFunctionType
AX = mybir.AxisListType

PE, ACT, DVE, POOL, SP = "tensor", "scalar", "vector", "gpsimd", "sync"
ENGS = (PE, ACT, DVE, POOL, SP)

D = 1024
NH = 16
DH = 64
NCOL = 3462
DFF = 4096
EPS = 1e-6
N_FOX = 6
MASKNEG = -1024.0


class Op:
    __slots__ = ("eng", "fn", "deps", "signal", "sem", "val", "is_dma")


class Buf:
    def __init__(self, name):
        self.name = name
        self.writers = []
        self.readers = []
        self.ld = None
        self.st = None


def _compress(lst):
    best = {}
    for o in lst:
        key = o.sem if o.is_dma else o.eng
        cur = best.get(key)
        if cur is None or o.val > cur.val:
            best[key] = o
    return list(best.values())


class Sched:
    def __init__(self, nc):
        self.nc = nc
        self.ops = {e: [] for e in ENGS}
        self.seq = 0
        self._semctx = []
        self.pool = {}
        self.live = []
        self.last_compute = {}
        self.dma_latest = {}
        self.engsem = {e: self.new_sem("prog_" + e) for e in ENGS}

    def new_sem(self, name):
        ctx = self.nc.semaphore(name)
        s = ctx.__enter__()
        self._semctx.append(ctx)
        return s

    def close(self):
        for ctx in reversed(self._semctx):
            ctx.__exit__(None, None, None)

    def _mk(self, eng, fn, reads, writes, pwrites):
        o = Op()
        o.eng, o.fn, o.signal, o.sem, o.is_dma = eng, fn, False, None, False
        self.seq += 1
        o.val = self.seq
        deps = []
        for b in reads:
            deps.extend(b.writers)
        for b in writes:
            deps.extend(b.writers)
            deps.extend(b.readers)
        for b in pwrites:
            deps.extend(b.writers)
            deps.extend(b.readers)
        o.deps = deps
        return o

    def _commit(self, o, reads, writes, pwrites):
        o.deps = _compress(o.deps)
        for b in reads:
            b.readers.append(o)
            if len(b.readers) > 12:
                b.readers = _compress(b.readers)
        for b in writes:
            b.writers = [o]
            b.readers = []
        for b in pwrites:
            b.writers.append(o)
            if len(b.writers) > 12:
                b.writers = _compress(b.writers)
        self.ops[o.eng].append(o)
        if o.is_dma:
            self.dma_latest[id(o.sem)] = o
        else:
            self.last_compute[o.eng] = o

    def op(self, eng, fn, reads=(), writes=(), pwrites=()):
        o = self._mk(eng, fn, reads, writes, pwrites)
        self._commit(o, reads, writes, pwrites)
        return o

    def _slot(self, sb, store, eng):
        slot = sb.st if store else sb.ld
        if slot is None:
            pool = self.pool.setdefault(eng, [])
            slot = pool.pop() if pool else [self.new_sem("dma%d" % len(self._semctx)), 0, None, eng]
            if store:
                sb.st = slot
            else:
                sb.ld = slot
            self.live.append(sb)
        return slot

    def dma(self, eng, fn, sb, store=False, reads=(), writes=(), pwrites=()):
        o = self._mk(eng, fn, reads, writes, pwrites)
        o.is_dma = True
        slot = self._slot(sb, store, eng)
        slot[1] += 16
        o.sem, o.val = slot[0], slot[1]
        if len(slot) > 2 and slot[2] is not None:
            o.deps.append(slot[2])
        if len(slot) > 2:
            slot[2] = o
        else:
            slot.append(o)
        self._commit(o, reads, writes, pwrites)
        return o

    def barrier(self):
        deps = list(self.last_compute.values()) + list(self.dma_latest.values())
        for e in ENGS:
            o = Op()
            o.eng, o.fn, o.signal, o.sem, o.is_dma = e, (lambda eng: None), False, None, False
            self.seq += 1
            o.val = self.seq
            o.deps = list(deps)
            self.ops[e].append(o)
        for b in self.live:
            if b.ld is not None:
                self.pool.setdefault(b.ld[3], []).append(b.ld)
                b.ld = None
            if b.st is not None:
                self.pool.setdefault(b.st[3], []).append(b.st)
                b.st = None
        self.live = []

    def emit(self, final_waits=()):
        nc = self.nc
        for e in ENGS:
            for o in self.ops[e]:
                for d in o.deps:
                    if not d.is_dma and (d.eng != o.eng or o.is_dma or d.eng != PE):
                        d.signal = True
        for e in ENGS:
            c = 0
            for o in self.ops[e]:
                if not o.is_dma and o.signal:
                    c += 1
                    o.val = c
                    o.sem = self.engsem[e]
        ops = self.ops

        def run(eng_name, e):
            waited = {}
            for o in ops[eng_name]:
                need = {}
                for d in o.deps:
                    if not d.is_dma and d.eng == o.eng and not o.is_dma and d.eng == PE:
                        continue
                    k = id(d.sem)
                    if k not in need or need[k][1] < d.val:
                        need[k] = (d.sem, d.val)
                for k, (sem, v) in need.items():
                    if waited.get(k, 0) < v:
                        e.wait_ge(sem, v)
                        waited[k] = v
                ins = o.fn(e)
                if ins is None:
                    continue
                if o.is_dma:
                    ins.then_inc(o.sem, 16)
                elif o.signal:
                    ins.then_inc(o.sem, 1)
            if eng_name == SP:
                for o in final_waits:
                    e.wait_ge(o.sem, o.val)

        with nc.allow_low_precision(reason="bf16 matmul operands are produced by fp32 math"), nc.Block() as block:
            @block.sync
            def _(e):
                run(SP, e)

            @block.tensor
            def _(e):
                run(PE, e)

            @block.scalar
            def _(e):
                run(ACT, e)

            @block.vector
            def _(e):
                run(DVE, e)

            @block.gpsimd
            def _(e):
                run(POOL, e)


def _split3(a):
    a = a.astype(np.float32)
    h = a.astype(ml_dtypes.bfloat16)
    r1 = a - h.astype(np.float32)
    m = r1.astype(ml_dtypes.bfloat16)
    r2 = r1 - m.astype(np.float32)
    lo = r2.astype(ml_dtypes.bfloat16)
    return np.stack([h, m, lo], axis=-2)


def make_consts(S):
    bf = ml_dtypes.bfloat16
    NT = S // 128
    c = {}
    c["ident"] = np.eye(128, dtype=np.float32).astype(bf)
    j = np.arange(128)[:, None]
    t = np.arange(128)[None, :]
    c["tri"] = np.where(j <= t, 0.0, -30000.0).astype(np.float32)
    band = np.zeros((128, 256), np.float32)
    band[:, 0:128] = np.where(j >= t, 0.0, -30000.0)
    band[:, 128:256] = np.where(j <= t, 0.0, -30000.0)
    c["band"] = np.concatenate([band, band], axis=1).astype(np.float32)
    onesf = np.zeros((65, 64), np.float32)
    onesf[64, :] = 1.0
    c["onesf"] = onesf
    blk = np.zeros((128, 128), np.float32)
    blk[0:64, 0:64] = 1.0 / 64
    blk[64:128, 64:128] = 1.0 / 64
    c["blk"] = blk.astype(bf)
    c["c65"] = np.ones((64, 1), np.float32).astype(bf)
    slopes = np.exp2(-8.0 * np.arange(1, 11, dtype=np.float32) / 10).astype(np.float32)
    hs = np.zeros(16, np.float32)
    hs[0:4] = slopes[6:10]
    hs[10:16] = slopes[0:6]
    pos = (hs[:, None] * np.arange(S, dtype=np.float32)[None, :]).astype(np.float32)
    c["posB"] = _split3(pos)
    c["posA"] = (-c["posB"].astype(np.float32)).astype(bf)
    c["ones3"] = np.ones((3, S), np.float32).astype(bf)
    n = np.arange(16)[:, None]
    c["ind16"] = ((np.arange(S)[None, :] // 256) == n).astype(np.float32).astype(bf)
    qb = (np.arange(NT) // 2)[:, None]
    nn = np.arange(16)[None, :]
    negm = np.where(nn < qb, 0.0, -1e30).astype(np.float32).reshape(1, NT * 16)
    past = (nn < qb).astype(np.float32).reshape(1, NT * 16)
    own = (nn == qb).astype(np.float32).reshape(1, NT * 16)
    c["negm"] = np.ascontiguousarray(np.broadcast_to(negm, (128, NT * 16)))
    c["past"] = np.ascontiguousarray(np.broadcast_to(past, (128, NT * 16)))
    c["own"] = np.ascontiguousarray(np.broadcast_to(own, (128, NT * 16)))
    return c


CONST_DT = {"ident": BF16, "tri": F32, "band": F32, "blk": BF16, "c65": BF16, "onesf": F32, "posA": BF16,
            "posB": BF16, "ones3": BF16, "ind16": BF16, "negm": F32, "past": F32, "own": F32}


def build(S=4096, NL=2, debug=False):
    NT = S // 128
    NCH = S // 512
    nc = bass.Bass("TRN2", target_bir_lowering=False)
    consts_np = make_consts(S)

    def din(name, shape, dt=F32):
        return nc.dram_tensor(name, list(shape), dt, kind="ExternalInput").ap()

    x_in = din("x", [S, D])
    attn_norm = din("attn_norm", [NL, D])
    w_in = din("w_in", [NL, D, NCOL])
    negb_in = din("b_forget", [NL, N_FOX, 1])
    qg_col = din("qg_col", [NL, 128, 8])
    kg_col = din("kg_col", [NL, 128, 8])
    og_col = din("og_col", [NL, 64, 16])
    w_out = din("w_out", [NL, D, D])
    mlp_norm = din("mlp_norm", [NL, D])
    w_up = din("w_up", [NL, D, DFF])
    w_down = din("w_down", [NL, DFF, D])
    cd = {k: din("c_" + k, v.shape, CONST_DT[k]) for k, v in consts_np.items()}
    out = nc.dram_tensor("out", [S, D], F32, kind="ExternalOutput").ap()
    skind = "ExternalOutput" if debug else "Internal"

    def dscr(name, shape, dt):
        return nc.dram_tensor(name, list(shape), dt, kind=skind).ap()

    QT = dscr("QT", [D, S], BF16)
    KT = dscr("KT", [D, S], BF16)
    V = dscr("V", [S, NH, 65], BF16)
    GT = dscr("GT", [N_FOX * 64, S], BF16)
    CUMA = dscr("CUMA", [N_FOX, 3, S], BF16)
    CUMB = dscr("CUMB", [N_FOX, 3, S], BF16)
    YT = dscr("YT", [D, S], BF16)
    X1 = dscr("X1", [S, D], F32)
    XM = dscr("XM", [S, D], F32) if debug else None
    DBGQ = dscr("DBGQ", [86, S], BF16) if debug else None
    bDBG = Buf("DBG")

    S_ = Sched(nc)
    bQT, bKT, bV, bGT, bCA, bCB, bYT, bX1, bOUT = [Buf(n) for n in
                                                   ("QT", "KT", "V", "GT", "CA", "CB", "YT", "X1", "OUT")]
    bXM = Buf("XM")

    with ExitStack() as top:
        uid = [0]

        def sbt(es, name, shape, dt):
            uid[0] += 1
            return es.enter_context(nc.sbuf_tensor("%s_u%d" % (name, uid[0]), list(shape), dt))

        pbank = [top.enter_context(nc.psum_tensor("pb%d" % i, [128, 512], F32)) for i in range(8)]
        bP = [Buf("pb%d" % i) for i in range(8)]

        ident = sbt(top, "ident", [128, 128], BF16)
        tri = sbt(top, "tri", [128, 128], F32)
        band = sbt(top, "band", [128, 512], F32)
        blk = sbt(top, "blk", [128, 128], BF16)
        c65 = sbt(top, "c65", [64, 1], BF16)
        onesf = sbt(top, "onesf", [65, 64], F32)
        SSQ = sbt(top, "SSQ", [128, NT, 3], F32)
        RG = sbt(top, "RG", [128, NT, 3], F32)
        bSSQ = Buf("SSQ")
        bRG = Buf("RG")
        epsb = sbt(top, "epsb", [128, 1], F32)
        oneb = sbt(top, "oneb", [128, 1], F32)
        qsb = sbt(top, "qsb", [128, 1], F32)
        zerob = sbt(top, "zerob", [128, 1], F32)
        bC = Buf("consts")
        for nm, t_ in (("ident", ident), ("tri", tri), ("band", band), ("blk", blk), ("c65", c65), ("onesf", onesf)):
            S_.dma(SP, (lambda e, t_=t_, nm=nm: e.dma_start(out=t_[:], in_=cd[nm])), bC, pwrites=[bC])
        S_.op(POOL, lambda e: e.memset(epsb[:], EPS), pwrites=[bC])
        S_.op(POOL, lambda e: e.memset(oneb[:], 1.0), pwrites=[bC])
        S_.op(POOL, lambda e: e.memset(qsb[:], float(-np.log(8.0))), pwrites=[bC])
        S_.op(POOL, lambda e: e.memset(zerob[:], 0.0), pwrites=[bC])

        def phase1(l, x_src, bx_src):
            with ExitStack() as es:
                w_sb = sbt(es, "w_in_sb", [128, 8, NCOL], BF16)
                bWs = [Buf("w_in%d" % i) for i in range(4)]
                for i in range(4):
                    S_.dma(POOL, (lambda e, i=i: e.dma_start(
                        out=w_sb[:, 2 * i:2 * i + 2, :],
                        in_=w_in[l, 256 * i:256 * (i + 1), :].rearrange("(k p) n -> p k n", p=128))),
                           bWs[i], writes=[bWs[i]])
                g1 = sbt(es, "g1", [128, D], F32)
                gq = sbt(es, "gq", [128, 16], F32)
                negb = sbt(es, "negb", [N_FOX, 1], F32)
                ones6 = sbt(es, "ones6", [N_FOX, 512], F32)
                bG = Buf("p1consts")
                S_.dma(SP, lambda e: e.dma_start(out=g1[:], in_=attn_norm[l:l + 1, :].partition_broadcast(128)),
                       bG, pwrites=[bG])
                S_.dma(SP, lambda e: e.dma_start(out=gq[:, 0:8], in_=qg_col[l]), bG, pwrites=[bG])
                S_.dma(SP, lambda e: e.dma_start(out=gq[:, 8:16], in_=kg_col[l]), bG, pwrites=[bG])
                S_.dma(SP, lambda e: e.dma_start(out=negb[:], in_=negb_in[l]), bG, pwrites=[bG])
                S_.op(DVE, lambda e: e.tensor_scalar(out=negb[:], in0=negb[:], scalar1=-1.0, scalar2=None,
                                                     op0=ALU.mult), reads=[bG], pwrites=[bG])
                S_.op(POOL, lambda e: e.memset(ones6[:], 1.0), pwrites=[bG])

                xt = [sbt(es, "xt%d" % i, [128, D], F32) for i in range(4)]
                bxt = [Buf("xt%d" % i) for i in range(4)]
                st8 = sbt(es, "st8", [128, 8], F32)
                bss = [Buf("ss%d" % i) for i in range(4)]
                brs = Buf("rs")
                st4 = [sbt(es, "st4_%d" % i, [128, 4], F32) for i in range(2)]
                bst = [Buf("st4_%d" % i) for i in range(2)]
                xs = [sbt(es, "xs%d" % i, [128, D], BF16) for i in range(4)]
                bxs = [Buf("xs%d" % i) for i in range(4)]
                hnT = [sbt(es, "hnT%d" % i, [128, 8, 512], BF16) for i in range(2)]
                bhn = [Buf("hnT%d" % i) for i in range(2)]
                sq = [sbt(es, "sq%d" % i, [128, 512], BF16) for i in range(2)]
                bsq = [Buf("sq%d" % i) for i in range(2)]
                lnt = [sbt(es, "lnt%d" % i, [128, 512], F32) for i in range(2)]
                blnt = [Buf("lnt%d" % i) for i in range(2)]
                qo = [sbt(es, "qo%d" % i, [128, 512], BF16) for i in range(3)]
                bqo = [Buf("qo%d" % i) for i in range(3)]
                vst = [sbt(es, "vst%d" % i, [128, NH, 65], BF16) for i in range(4)]
                bvst = [Buf("vst%d" % i) for i in range(4)]
                for i in range(4):
                    S_.op(POOL, (lambda e, i=i: e.memset(vst[i][:, :, 64:65], 1.0)), pwrites=[bvst[i]])
                cn = [sbt(es, "cn%d" % i, [N_FOX, 512], F32) for i in range(2)]
                bcn = [Buf("cn%d" % i) for i in range(2)]
                fr = sbt(es, "fr", [N_FOX, 512], F32)
                bfr = Buf("fr")
                fr2 = sbt(es, "fr2", [N_FOX, 512], F32)
                bfr2 = Buf("fr2")
                pa3 = [sbt(es, "pa3_%d" % i, [N_FOX, 3, 512], BF16) for i in range(2)]
                pb3 = [sbt(es, "pb3_%d" % i, [N_FOX, 3, 512], BF16) for i in range(2)]
                bp3 = [Buf("p3_%d" % i) for i in range(2)]

                def loadX(c):
                    for tt in range(4):
                        t0 = c * 512 + tt * 128
                        S_.dma(SP, (lambda e, tt=tt, t0=t0: e.dma_start(out=xt[tt][:], in_=x_src[t0:t0 + 128, :])),
                               bxt[tt], reads=[bx_src], writes=[bxt[tt]])

                def stageA1(c):
                    for tt in range(4):
                        S_.op(ACT, (lambda e, tt=tt: e.activation(out=xs[tt][:], in_=xt[tt][:], func=AF.Square)),
                              reads=[bxt[tt]], writes=[bxs[tt]])
                    for tt in range(4):
                        S_.op(DVE, (lambda e, tt=tt: e.tensor_reduce(out=st8[:, tt:tt + 1], in_=xs[tt][:], axis=AX.X,
                                                                     op=ALU.add)),
                              reads=[bxs[tt]], writes=[bss[tt]])
                    S_.op(ACT, lambda e: e.activation(out=st8[:, 4:8], in_=st8[:, 0:4], func=AF.Ln, bias=epsb[:],
                                                      scale=1.0 / D),
                          reads=bss + [bC], writes=[brs])
                    S_.op(ACT, lambda e: e.activation(out=st8[:, 4:8], in_=st8[:, 4:8], func=AF.Exp, scale=-0.5),
                          writes=[brs])
                    for tt in range(4):
                        S_.op(DVE, (lambda e, tt=tt: e.scalar_tensor_tensor(out=xs[tt][:], in0=xt[tt][:],
                                                                            scalar=st8[:, 4 + tt:5 + tt], in1=g1[:],
                                                                            op0=ALU.mult, op1=ALU.mult)),
                              reads=[bxt[tt], brs, bG], writes=[bxs[tt]])

                def stageA2(c, tt):
                    hb = c % 2
                    k4 = tt
                    pbf = pbank[0][:].bitcast(BF16)

                    def tr(e):
                        ins = None
                        for dc in range(8):
                            ins = e.transpose(pbf[:, dc * 128:(dc + 1) * 128], xs[k4][:, dc * 128:(dc + 1) * 128],
                                              ident[:])
                        return ins
                    S_.op(PE, tr, reads=[bxs[k4], bC], writes=[bP[0]])
                    S_.op(DVE, lambda e: e.tensor_copy(out=hnT[hb][:, :, tt * 128:(tt + 1) * 128],
                                                       in_=pbf.rearrange("p (c t) -> p c t", c=8)),
                          reads=[bP[0]], pwrites=[bhn[hb]])

                def proj(hb, col0, ncols, pidx, tcols=slice(0, 512)):
                    def f(e):
                        ins = None
                        for dc in range(8):
                            ins = e.matmul(pbank[pidx][0:ncols, :], lhsT=w_sb[:, dc, col0:col0 + ncols],
                                           rhs=hnT[hb][:, dc, :], start=(dc == 0), stop=(dc == 7))
                        return ins
                    S_.op(PE, f, reads=bWs + [bhn[hb]], writes=[bP[pidx]])

                cnt = {"qo": 0, "sq": 0}

                def stageB(c):
                    hb = c % 2
                    cs = slice(c * 512, (c + 1) * 512)
                    if c + 1 < NCH:
                        stageA1(c + 1)
                    PA = (1, 2, 5)
                    proj(hb, 0, 128, PA[0])
                    for j in range(16):
                        pa = PA[j % 3]
                        pm = 3 + (j % 2)
                        if c + 1 < NCH and j % 4 == 2:
                            stageA2(c + 1, j // 4)
                        if j + 1 < 16:
                            proj(hb, (j + 1) * 128, 128, PA[(j + 1) % 3])
                        si = cnt["sq"] % 2
                        cnt["sq"] += 1
                        S_.op(ACT, (lambda e, pa=pa, si=si: e.activation(out=sq[si][:], in_=pbank[pa][:],
                                                                         func=AF.Square)),
                              reads=[bP[pa]], writes=[bsq[si]])
                        S_.op(PE, (lambda e, pm=pm, si=si: e.matmul(pbank[pm][:], lhsT=blk[:], rhs=sq[si][:],
                                                                    start=True, stop=True)),
                              reads=[bsq[si], bC], writes=[bP[pm]])
                        S_.op(ACT, (lambda e, pm=pm, si=si: e.activation(out=lnt[si][:], in_=pbank[pm][:],
                                                                         func=AF.Ln, bias=epsb[:])),
                              reads=[bP[pm], bC], writes=[blnt[si]])
                        bias_t = qsb if j < 8 else zerob
                        S_.op(ACT, (lambda e, si=si, bias_t=bias_t: e.activation(out=lnt[si][:], in_=lnt[si][:],
                                                                                 func=AF.Exp, scale=-0.5,
                                                                                 bias=bias_t[:])),
                              reads=[bC], writes=[blnt[si]])
                        qi = cnt["qo"] % 3
                        cnt["qo"] += 1
                        S_.op(DVE, (lambda e, pa=pa, si=si, qi=qi, j=j: e.scalar_tensor_tensor(
                            out=qo[qi][:], in0=pbank[pa][:], scalar=gq[:, j:j + 1], in1=lnt[si][:],
                            op0=ALU.mult, op1=ALU.mult)),
                              reads=[bP[pa], blnt[si], bG], writes=[bqo[qi]])
                        dst, bdst = (QT, bQT) if j < 8 else (KT, bKT)
                        r0 = (j % 8) * 128
                        S_.dma(SP, (lambda e, qi=qi, dst=dst, r0=r0: e.dma_start(out=dst[r0:r0 + 128, cs],
                                                                                 in_=qo[qi][:])),
                               bqo[qi], store=True, reads=[bqo[qi]], pwrites=[bdst])
                    for i in range(3):
                        pa = 1 + (i % 2)
                        proj(hb, 3078 + i * 128, 128, pa)
                        si = cnt["sq"] % 2
                        cnt["sq"] += 1
                        S_.op(ACT, (lambda e, pa=pa, si=si: e.activation(out=lnt[si][:], in_=pbank[pa][:],
                                                                         func=AF.Exp, scale=-1.0)),
                              reads=[bP[pa]], writes=[blnt[si]])
                        S_.op(DVE, (lambda e, si=si: e.tensor_scalar(out=lnt[si][:], in0=lnt[si][:], scalar1=1.0,
                                                                     scalar2=None, op0=ALU.add)),
                              writes=[blnt[si]])
                        qi = cnt["qo"] % 3
                        cnt["qo"] += 1
                        S_.op(DVE, (lambda e, si=si, qi=qi: e.reciprocal(out=qo[qi][:], in_=lnt[si][:])),
                              reads=[blnt[si]], writes=[bqo[qi]])
                        S_.dma(SP, (lambda e, qi=qi, i=i: e.dma_start(out=GT[i * 128:(i + 1) * 128, cs],
                                                                      in_=qo[qi][:])),
                               bqo[qi], store=True, reads=[bqo[qi]], pwrites=[bGT])
                    proj(hb, 3072, N_FOX, 7)
                    S_.op(ACT, lambda e: e.activation(out=fr[:], in_=pbank[7][0:N_FOX, :], func=AF.Exp,
                                                      scale=-1.0, bias=negb[:]),
                          reads=[bP[7], bG], writes=[bfr])
                    S_.op(ACT, lambda e: e.activation(out=fr[:], in_=fr[:], func=AF.Ln, bias=oneb[0:N_FOX, :]),
                          reads=[bC], writes=[bfr])
                    ci = c % 2
                    init = 0.0 if c == 0 else cn[1 - ci][:, 511:512]
                    S_.op(DVE, (lambda e, ci=ci, init=init: e.tensor_tensor_scan(
                        out=cn[ci][:], data0=ones6[:], data1=fr[:], initial=init, op0=ALU.mult, op1=ALU.add)),
                          reads=[bfr, bG, bcn[1 - ci]], writes=[bcn[ci]])
                    S_.op(DVE, (lambda e, ci=ci: e.tensor_copy(out=pb3[ci][:, 0, :], in_=cn[ci][:])),
                          reads=[bcn[ci]], writes=[bp3[ci]])
                    S_.op(DVE, (lambda e, ci=ci: e.tensor_tensor(out=fr[:], in0=cn[ci][:], in1=pb3[ci][:, 0, :],
                                                                 op=ALU.subtract)),
                          reads=[bcn[ci], bp3[ci]], writes=[bfr])
                    S_.op(DVE, (lambda e, ci=ci: e.tensor_copy(out=pb3[ci][:, 1, :], in_=fr[:])),
                          reads=[bfr], pwrites=[bp3[ci]])
                    S_.op(DVE, (lambda e, ci=ci: e.tensor_tensor(out=fr2[:], in0=fr[:], in1=pb3[ci][:, 1, :],
                                                                 op=ALU.subtract)),
                          reads=[bfr, bp3[ci]], writes=[bfr2])
                    S_.op(DVE, (lambda e, ci=ci: e.tensor_copy(out=pb3[ci][:, 2, :], in_=fr2[:])),
                          reads=[bfr2], pwrites=[bp3[ci]])
                    S_.op(DVE, (lambda e, ci=ci: e.tensor_scalar(out=pa3[ci][:], in0=pb3[ci][:], scalar1=-1.0,
                                                                 scalar2=None, op0=ALU.mult)),
                          reads=[bp3[ci]], pwrites=[bp3[ci]])
                    S_.dma(SP, (lambda e, ci=ci: e.dma_start(out=CUMA[:, :, cs], in_=pa3[ci][:])),
                           bp3[ci], store=True, reads=[bp3[ci]], pwrites=[bCA])
                    S_.dma(SP, (lambda e, ci=ci: e.dma_start(out=CUMB[:, :, cs], in_=pb3[ci][:])),
                           bp3[ci], store=True, reads=[bp3[ci]], pwrites=[bCB])
                    for tt in range(4):
                        vi = tt
                        t0 = c * 512 + tt * 128
                        for half in range(2):
                            pv = 5 + half

                            def f(e, pv=pv, tt=tt, half=half):
                                ins = None
                                for dc in range(8):
                                    ins = e.matmul(pbank[pv][:], lhsT=hnT[hb][:, dc, tt * 128:(tt + 1) * 128],
                                                   rhs=w_sb[:, dc, 2048 + half * 512:2048 + (half + 1) * 512],
                                                   start=(dc == 0), stop=(dc == 7))
                                return ins
                            S_.op(PE, f, reads=bWs + [bhn[hb]], writes=[bP[pv]])
                            S_.op(ACT, (lambda e, pv=pv, vi=vi, half=half: e.copy(
                                out=vst[vi][:, half * 8:(half + 1) * 8, 0:64],
                                in_=pbank[pv][:].rearrange("p (h d) -> p h d", h=8))),
                                  reads=[bP[pv]], pwrites=[bvst[vi]])
                        S_.dma(SP, (lambda e, vi=vi, t0=t0: e.dma_start(out=V[t0:t0 + 128, :, :], in_=vst[vi][:])),
                               bvst[vi], store=True, reads=[bvst[vi]], pwrites=[bV])

                loadX(0)
                stageA1(0)
                for tt in range(4):
                    stageA2(0, tt)
                if NCH > 1:
                    loadX(1)
                for c in range(NCH):
                    stageB(c)
                    if c + 2 < NCH:
                        loadX(c + 2)
            S_.barrier()

        def phase2(l):
            with ExitStack() as es:
                ogc = sbt(es, "ogc", [64, 16], F32)
                negm = sbt(es, "negm", [128, NT * 16], F32)
                past = sbt(es, "past", [128, NT * 16], F32)
                own = sbt(es, "own", [128, NT * 16], F32)
                bK = Buf("p2consts")
                S_.dma(SP, lambda e: e.dma_start(out=ogc[:], in_=og_col[l]), bK, pwrites=[bK])
                for nm, t_ in (("negm", negm), ("past", past), ("own", own)):
                    S_.dma(SP, (lambda e, t_=t_, nm=nm: e.dma_start(out=t_[:], in_=cd[nm])), bK, pwrites=[bK])
                RMAX = 86
                qa = [sbt(es, "qa%d" % i, [RMAX, S], BF16) for i in range(2)]
                ka = [sbt(es, "ka%d" % i, [RMAX, S], BF16) for i in range(2)]
                bqa = [Buf("qa%d" % i) for i in range(2)]
                bka = [Buf("ka%d" % i) for i in range(2)]
                va = [sbt(es, "va%d" % i, [128, NT, 65], BF16) for i in range(2)]
                bva = [Buf("va%d" % i) for i in range(2)]
                vd = [[sbt(es, "vd%d_%d" % (j, i), [128, NT, 65], BF16) for i in range(2)] for j in range(2)]
                bvd = [[[Buf("vd%d_%d_%d" % (j, i, r)) for r in range(dd)] for i, dd in enumerate((4, 16))]
                       for j in range(2)]
                pT = [sbt(es, "pT%d" % i, [128, 512], BF16) for i in range(4)]
                bpT = [Buf("pT%d" % i) for i in range(4)]
                sq65 = sbt(es, "sq65", [65, 512], BF16)
                bsq65 = Buf("sq65")
                rden = sbt(es, "rden", [65, 512], F32)
                brden = Buf("rden")
                t0 = sbt(es, "t0", [64, 512], F32)
                bt0 = Buf("t0")
                S_.op(POOL, lambda e: e.memset(SSQ[:], 0.0), writes=[bSSQ])
                lnf = sbt(es, "lnf", [64, 512], F32)
                blnf = Buf("lnf")
                yf = [sbt(es, "yf%d" % i, [64, 512], BF16) for i in range(2)]
                byf = [Buf("yf%d" % i) for i in range(2)]
                gt = [sbt(es, "gt%d" % i, [64, 512], BF16) for i in range(2)]
                bgt = [Buf("gt%d" % i) for i in range(2)]
                kms = sbt(es, "kms", [64, 16], F32)
                kmb = sbt(es, "kmb", [64, 16], BF16)
                bkm = Buf("km")
                gm2 = [sbt(es, "gm%d" % i, [128, 16], F32) for i in range(3)]
                top82 = [sbt(es, "top8%d" % i, [128, 8], F32) for i in range(3)]
                t12 = [sbt(es, "t1%d" % i, [128, 16], F32) for i in range(3)]
                t22 = [sbt(es, "t2%d" % i, [128, 16], F32) for i in range(3)]
                bgm2, btop82, bt12, bt22 = [[Buf("%s%d" % (n, i)) for i in range(3)] for n in ("gm", "top8", "t1", "t2")]
                zp = [sbt(es, "zp%d" % i, [128, 80], BF16) for i in range(3)]
                bzp = [Buf("zp%d" % i) for i in range(3)]
                zero1 = sbt(es, "zero1", [1, 128], BF16)
                for i in range(3):
                    S_.op(POOL, (lambda e, i=i: e.memset(zp[i][:], 0.0)), writes=[bzp[i]])
                S_.op(POOL, lambda e: e.memset(zero1[:], 0.0), pwrites=[bK])
                ycnt = [0]

                def finalize(h, pob, c, gated_gi=None):
                    cs = slice(c * 512, (c + 1) * 512)
                    g = 0 if h < 4 else (1 if h < 10 else 2)
                    def partA():
                        S_.op(ACT, lambda e: e.activation(out=rden[64:65, :], in_=pbank[pob][64:65, :], func=AF.Ln),
                              reads=[bP[pob]], writes=[brden])

                    def partB():
                        S_.op(PE, lambda e: e.matmul(pbank[7][0:64, :], lhsT=onesf[64:65, :], rhs=rden[64:65, :],
                                                     start=True, stop=True),
                              reads=[brden, bC], writes=[bP[7]])
                        S_.op(ACT, lambda e: e.activation(out=lnf[:], in_=pbank[7][0:64, :], func=AF.Exp, scale=-1.0),
                              reads=[bP[7]], writes=[blnf])
                        S_.op(DVE, lambda e: e.tensor_tensor(out=t0[:], in0=pbank[pob][0:64, :], in1=lnf[:],
                                                             op=ALU.mult),
                              reads=[bP[pob], blnf], writes=[bt0])
                        S_.op(ACT, lambda e: e.activation(out=sq65[0:64, :], in_=t0[:], func=AF.Square),
                              reads=[bt0], writes=[bsq65])
                        yi = ycnt[0] % 2
                        ycnt[0] += 1
                        S_.op(DVE, lambda e: e.tensor_scalar(out=yf[yi][:], in0=t0[:], scalar1=ogc[:, h:h + 1],
                                                             scalar2=None, op0=ALU.mult),
                              reads=[bt0, bK], writes=[byf[yi]])
                        if gated_gi is not None:
                            S_.op(POOL, lambda e: e.tensor_tensor(out=yf[yi][:], in0=yf[yi][:],
                                                                  in1=gt[gated_gi][:], op=ALU.mult),
                                  reads=[bgt[gated_gi]], writes=[byf[yi]])
                        S_.dma(SP, lambda e: e.dma_start(out=YT[h * 64:(h + 1) * 64, cs], in_=yf[yi][:]),
                               byf[yi], store=True, reads=[byf[yi]], pwrites=[bYT])

                    def partC():
                        def fss(e):
                            ins = None
                            for i in range(4):
                                ins = e.matmul(pbank[7][:, i:i + 1], lhsT=sq65[0:64, i * 128:(i + 1) * 128],
                                               rhs=c65[:], start=True, stop=True)
                            return ins
                        S_.op(PE, fss, reads=[bsq65, bC], writes=[bP[7]])
                        S_.op(DVE, lambda e: e.tensor_tensor(out=SSQ[:, 4 * c:4 * c + 4, g],
                                                             in0=SSQ[:, 4 * c:4 * c + 4, g],
                                                             in1=pbank[7][:, 0:4], op=ALU.add),
                              reads=[bP[7]], pwrites=[bSSQ])
                    deferred.append([1, partA])
                    deferred.append([3, partB])
                    deferred.append([6, partC])

                def load_head(h, hi, kind):
                    S_.dma(SP, lambda e: e.dma_start(out=qa[hi][0:64, :], in_=QT[h * 64:(h + 1) * 64, :]),
                           bqa[hi], reads=[bQT], writes=[bqa[hi]])
                    S_.dma(SP, lambda e: e.dma_start(out=ka[hi][0:64, :], in_=KT[h * 64:(h + 1) * 64, :]),
                           bka[hi], reads=[bKT], writes=[bka[hi]])
                    if kind == "fox":
                        hf = h - 4
                        r = 64
                        S_.dma(SP, lambda e: e.dma_start(out=qa[hi][r:r + 3, :], in_=CUMA[hf]), bqa[hi],
                               reads=[bCA], pwrites=[bqa[hi]])
                        S_.dma(SP, lambda e: e.dma_start(out=ka[hi][r + 3:r + 6, :], in_=CUMB[hf]), bka[hi],
                               reads=[bCB], pwrites=[bka[hi]])
                    else:
                        r = 80 if kind == "moba" else 64
                        S_.dma(SP, lambda e: e.dma_start(out=qa[hi][r:r + 3, :], in_=cd["posA"][h]), bqa[hi],
                               pwrites=[bqa[hi]])
                        S_.dma(SP, lambda e: e.dma_start(out=ka[hi][r + 3:r + 6, :], in_=cd["posB"][h]), bka[hi],
                               pwrites=[bka[hi]])
                    S_.dma(SP, lambda e: e.dma_start(out=qa[hi][r + 3:r + 6, :], in_=cd["ones3"]), bqa[hi],
                           pwrites=[bqa[hi]])
                    S_.dma(SP, lambda e: e.dma_start(out=ka[hi][r:r + 3, :], in_=cd["ones3"]), bka[hi],
                           pwrites=[bka[hi]])
                    if kind == "moba":
                        S_.dma(SP, lambda e: e.dma_start(out=ka[hi][64:80, :], in_=cd["ind16"]), bka[hi],
                               pwrites=[bka[hi]])

                def load_v(h, vt, bvts_, d):
                    for r in range(d):
                        src = V.rearrange("(b p r) h e -> p b r h e", p=128, r=d)[:, :, r, h, :]
                        dst = vt[:].rearrange("p (b r) e -> p b r e", r=d)[:, :, r, :]
                        S_.dma(SP, (lambda e, src=src, dst=dst: e.dma_start(out=dst, in_=src)), bvts_[r], reads=[bV],
                               writes=[bvts_[r]])

                def moba_prelude_items(h, hi):
                    def head_part():
                        S_.op(DVE, lambda e: e.memset(kms[:], 0.0), writes=[bkm])
                        S_.op(DVE, lambda e: e.tensor_reduce(out=kms[:, 0:S // 256],
                                                             in_=ka[hi][0:64, :].rearrange("p (n k) -> p n k", k=256),
                                                             axis=AX.X, op=ALU.add),
                              reads=[bka[hi]], writes=[bkm])
                        S_.op(DVE, lambda e: e.tensor_copy(out=kmb[:], in_=kms[:]), writes=[bkm])

                    def p1(i):
                        zi = i % 3
                        sl = slice(i * 16, (i + 1) * 16)
                        gc = slice((i % 8) * 16, (i % 8) * 16 + 16)
                        gm, top8, t1, t2 = gm2[zi], top82[zi], t12[zi], t22[zi]
                        bgm, btop8, bt1, bt2 = bgm2[zi], btop82[zi], bt12[zi], bt22[zi]
                        S_.op(PE, lambda e: e.matmul(pbank[6][:, gc], lhsT=qa[hi][0:64, i * 128:(i + 1) * 128],
                                                     rhs=kmb[:], start=True, stop=True),
                              reads=[bqa[hi], bkm], writes=[bP[6]])
                        S_.op(DVE, lambda e: e.tensor_tensor(out=gm[:], in0=pbank[6][:, gc], in1=negm[:, sl],
                                                             op=ALU.add),
                              reads=[bP[6], bK], writes=[bgm])
                        S_.op(DVE, lambda e: e.max(out=top8[:], in_=gm[:]), reads=[bgm], writes=[btop8])
                        S_.op(DVE, lambda e: e.scalar_tensor_tensor(out=t1[:], in0=gm[:], scalar=top8[:, 2:3],
                                                                    in1=past[:, sl], op0=ALU.is_ge, op1=ALU.mult),
                              reads=[bgm, btop8, bK], writes=[bt1])
                        S_.op(DVE, lambda e: e.tensor_tensor(out=t2[:], in0=t1[:], in1=own[:, sl], op=ALU.add),
                              reads=[bt1, bK], writes=[bt2])
                        S_.op(DVE, lambda e: e.tensor_scalar(out=zp[zi][:, 64:80], in0=t2[:], scalar1=-1.0,
                                                             scalar2=-MASKNEG, op0=ALU.add, op1=ALU.mult),
                              reads=[bt2], pwrites=[bzp[zi]])

                    def p2(i):
                        zi = i % 3
                        mc = slice((i % 2) * 128, (i % 2) * 128 + 128)
                        S_.op(PE, lambda e: e.matmul(pbank[7][0:80, mc], lhsT=zp[zi][:, 0:80], rhs=ident[:],
                                                     start=True, stop=True),
                              reads=[bzp[zi], bC], writes=[bP[7]])
                        S_.op(ACT, lambda e: e.copy(out=qa[hi][64:80, i * 128:(i + 1) * 128], in_=pbank[7][64:80, mc]),
                              reads=[bP[7]], pwrites=[bqa[hi]])

                    def item(k):
                        def f():
                            if k - 2 >= 0:
                                p2(k - 2)
                            if k < NT:
                                p1(k)
                        return f
                    return [head_part] + [item(k) for k in range(NT + 2)]

                LOOK = 2
                deferred = []

                def tick():
                    keep = []
                    for item in deferred:
                        item[0] -= 1
                        if item[0] <= 0:
                            item[1]()
                        else:
                            keep.append(item)
                    deferred[:] = keep

                def flush():
                    while deferred:
                        tick()

                bg = []

                def run_pipeline(steps, LOOK=2):
                    n = len(steps)
                    for i in range(min(LOOK, n)):
                        steps[i][0]()
                    for k in range(n):
                        if k + LOOK < n:
                            steps[k + LOOK][0]()
                        steps[k][1]()
                        tick()
                        if bg and k % 4 == 1:
                            bg.pop(0)()
                    while bg:
                        bg.pop(0)()

                def causal_head(h, hi, kind):
                    R = 86 if kind == "moba" else 70
                    if debug and h == 0:
                        S_.dma(SP, lambda e: e.dma_start(out=DBGQ, in_=qa[hi][0:86, :]), bDBG, store=True,
                               reads=[bqa[hi]], pwrites=[bDBG])
                    steps = []
                    k = 0
                    for c in range(NCH):
                        for jt in range(4 * c + 4):
                            steps.append(mk_causal_step(h, hi, kind, R, c, jt, k))
                            k += 1
                    run_pipeline(steps, LOOK=3)

                def mk_causal_step(h, hi, kind, R, c, jt, k):
                    njt = 4 * c + 4
                    m = jt - 4 * c
                    off = max(m, 0) * 128
                    ps = k % 4
                    po = 4 + (c % 2)

                    def qk():
                        S_.op(PE, lambda e: e.matmul(pbank[ps][:, off:512], lhsT=ka[hi][0:R, jt * 128:(jt + 1) * 128],
                                                     rhs=qa[hi][0:R, c * 512 + off:(c + 1) * 512],
                                                     start=True, stop=True),
                              reads=[bqa[hi], bka[hi]], writes=[bP[ps]])

                    def rest():
                        if jt == 0 and kind == "fox":
                            gi = c % 2
                            S_.dma(SP, lambda e: e.dma_start(
                                out=gt[gi][:], in_=GT[(h - 4) * 64:(h - 3) * 64, c * 512:(c + 1) * 512]),
                                   bgt[gi], reads=[bGT], writes=[bgt[gi]])
                        if m >= 0:
                            S_.op(DVE, lambda e: e.tensor_tensor(
                                out=pbank[ps][:, off:off + 128], in0=pbank[ps][:, off:off + 128], in1=tri[:],
                                op=ALU.add), reads=[bC], writes=[bP[ps]])
                        S_.op(ACT, lambda e: e.activation(out=pT[ps][:, off:512], in_=pbank[ps][:, off:512],
                                                          func=AF.Exp),
                              reads=[bP[ps]], writes=[bpT[ps]])
                        S_.op(PE, lambda e: e.matmul(pbank[po][0:65, off:512], lhsT=va[hi][:, jt, :],
                                                     rhs=pT[ps][:, off:512], start=(jt == 0), stop=(jt == njt - 1)),
                              reads=[bpT[ps], bva[hi]], writes=[bP[po]] if jt == 0 else (),
                              pwrites=() if jt == 0 else [bP[po]])
                        if jt == njt - 1:
                            finalize(h, po, c, gated_gi=(c % 2) if kind == "fox" else None)
                    return (qk, rest)

                def dil_head(h, hi):
                    R = 70
                    dils = (1, 4, 16)
                    vts = (va[hi], vd[hi][0], vd[hi][1])
                    bvts = ([bva[hi]], bvd[hi][0], bvd[hi][1])
                    SC = min(2048, S)
                    nq = SC // 512
                    kk = [0]
                    for sc in range(S // SC):
                        flush()
                        for q in range(nq):
                            S_.op(PE, (lambda e, q=q: e.matmul(pbank[3 + q][0:65, :], lhsT=zero1[0:1, 0:65],
                                                               rhs=qa[hi][0:1, 0:512], start=True, stop=False)),
                                  reads=[bK, bqa[hi]], writes=[bP[3 + q]])
                        blocks = []
                        for di, d in enumerate(dils):
                            nblk = SC // (d * 128)
                            for r in range(d):
                                for b in range(sc * nblk, (sc + 1) * nblk):
                                    blocks.append((di, d, r, b))
                        steps = []
                        for i in range(0, len(blocks), 2):
                            steps.append(mk_dil_step(h, hi, R, sc, SC, nq, blocks[i:i + 2], kk[0], vts, bvts))
                            kk[0] += 1
                        run_pipeline(steps)
                        for q in range(nq):
                            S_.op(PE, (lambda e, q=q: e.matmul(pbank[3 + q][0:65, :], lhsT=zero1[0:1, 0:65],
                                                               rhs=qa[hi][0:1, 0:512], start=False, stop=True)),
                                  reads=[bK, bqa[hi]], pwrites=[bP[3 + q]])
                        for q in range(nq):
                            finalize(h, 3 + q, (sc * SC) // 512 + q)
                            flush()

                def mk_dil_step(h, hi, R, sc, SC, nq, blks, k, vts, bvts):
                    ps = k % 3
                    W = 256 * len(blks)

                    def qk():
                        def f(e):
                            ins = None
                            for i, (di, d, r, b) in enumerate(blks):
                                qv = qa[hi][0:R, :].rearrange("p (i d) -> p d i", d=d)
                                kv = ka[hi][0:R, :].rearrange("p (i d) -> p d i", d=d)
                                for s_, kb in ((0, b - 1), (1, b)):
                                    if kb < 0:
                                        ins = e.matmul(pbank[ps][:, i * 256:i * 256 + 128], lhsT=zero1[0:1, :],
                                                       rhs=qv[0:1, r, b * 128:(b + 1) * 128], start=True, stop=True)
                                        continue
                                    ins = e.matmul(pbank[ps][:, i * 256 + s_ * 128:i * 256 + (s_ + 1) * 128],
                                                   lhsT=kv[:, r, kb * 128:(kb + 1) * 128],
                                                   rhs=qv[:, r, b * 128:(b + 1) * 128], start=True, stop=True)
                            return ins
                        S_.op(PE, f, reads=[bqa[hi], bka[hi], bK], writes=[bP[ps]])

                    def rest():
                        S_.op(DVE, lambda e: e.tensor_tensor(out=pbank[ps][:, 0:W], in0=pbank[ps][:, 0:W],
                                                             in1=band[:, 0:W], op=ALU.add),
                              reads=[bC], writes=[bP[ps]])
                        S_.op(ACT, lambda e: e.activation(out=pT[ps][:, 0:W], in_=pbank[ps][:, 0:W], func=AF.Exp),
                              reads=[bP[ps]], writes=[bpT[ps]])
                        banks = set()
                        plan = []
                        for i, (di, d, r, b) in enumerate(blks):
                            tstart = r + d * b * 128 - sc * SC
                            span = d * 128
                            segs = []
                            if span <= 512:
                                q = tstart // 512
                                base = tstart - q * 512
                                if d == 1:
                                    oview = pbank[3 + q][0:65, base:base + 128]
                                else:
                                    oview = pbank[3 + q][0:65, :].rearrange("p (u d) -> p d u", d=d)[:, r, :]
                                segs.append((oview, 0, 128))
                                banks.add(3 + q)
                            else:
                                per = 512 // d
                                for q in range(nq):
                                    oview = pbank[3 + q][0:65, :].rearrange("p (u d) -> p d u", d=d)[:, r, :]
                                    segs.append((oview, q * per, per))
                                    banks.add(3 + q)
                            slots = [(1, b)] if b == 0 else [(0, b - 1), (1, b)]
                            plan.append((i, di, d, r, slots, segs))

                        def fpv(e):
                            ins = None
                            for (i, di, d, r, slots, segs) in plan:
                                for (s_, kb) in slots:
                                    for (oview, u0, n) in segs:
                                        c0 = i * 256 + s_ * 128 + u0
                                        ins = e.matmul(oview, lhsT=vts[di][:, kb * d + r, :], rhs=pT[ps][:, c0:c0 + n],
                                                       start=False, stop=False)
                            return ins
                        S_.op(PE, fpv, reads=[bpT[ps]] + [bvts[p[1]][p[3] if p[1] > 0 else 0] for p in plan],
                              pwrites=[bP[x] for x in sorted(banks)])
                    return (qk, rest)

                heads = [(h, "moba") for h in range(0, 4)] + [(h, "fox") for h in range(4, 10)] + \
                        [(h, "dil") for h in range(10, 16)]
                def load_all(idx):
                    h, kind = heads[idx]
                    hi = idx % 2
                    load_head(h, hi, kind)
                    load_v(h, va[hi], [bva[hi]], 1)
                    if kind == "dil":
                        load_v(h, vd[hi][0], bvd[hi][0], 4)
                        load_v(h, vd[hi][1], bvd[hi][1], 16)

                load_all(0)
                for idx, (h, kind) in enumerate(heads):
                    hi = idx % 2
                    if idx + 1 < len(heads):
                        load_all(idx + 1)
                    if kind == "moba" and idx == 0:
                        for it in moba_prelude_items(h, hi):
                            it()
                    if idx + 1 < len(heads) and heads[idx + 1][1] == "moba":
                        bg.extend(moba_prelude_items(heads[idx + 1][0], (idx + 1) % 2))
                    if kind == "dil":
                        dil_head(h, hi)
                    else:
                        causal_head(h, hi, kind)
                flush()
                for g, W in ((0, 256), (1, 384), (2, 384)):
                    S_.op(ACT, (lambda e, g=g, W=W: e.activation(out=RG[:, :, g], in_=SSQ[:, :, g], func=AF.Ln,
                                                                 bias=epsb[:], scale=1.0 / W)),
                          reads=[bSSQ, bC], pwrites=[bRG])
                    S_.op(ACT, (lambda e, g=g: e.activation(out=RG[:, :, g], in_=RG[:, :, g], func=AF.Exp,
                                                            scale=-0.5)), pwrites=[bRG])
            S_.barrier()

        def load_w3a(l, es):
            wo = sbt(es, "wo", [128, 8, D], BF16)
            wu = sbt(es, "wu", [128, 8, DFF], BF16)
            bwo = [Buf("wo%d" % i) for i in range(2)]
            bwu = [Buf("wu%d" % i) for i in range(4)]
            for i in range(2):
                S_.dma(POOL, (lambda e, i=i: e.dma_start(
                    out=wo[:, 4 * i:4 * i + 4, :],
                    in_=w_out[l, 512 * i:512 * (i + 1), :].rearrange("(k p) n -> p k n", p=128))),
                       bwo[i], writes=[bwo[i]])
            for i in range(4):
                S_.dma(POOL, (lambda e, i=i: e.dma_start(
                    out=wu[:, 2 * i:2 * i + 2, :],
                    in_=w_up[l, 256 * i:256 * (i + 1), :].rearrange("(k p) n -> p k n", p=128))),
                       bwu[i], writes=[bwu[i]])
            return wo, wu, bwo, bwu

        def phase3(l, x_src, bx_src, x_dst, bx_dst, w3a):
            TC = 256
            wo, wu, bwo, bwu = w3a
            with ExitStack() as es:
                wd = sbt(es, "wd", [128, 32, D], BF16)
                bwd = [Buf("wd%d" % i) for i in range(4)]
                for i in range(4):
                    S_.dma(POOL, (lambda e, i=i: e.dma_start(
                        out=wd[:, 8 * i:8 * i + 8, :],
                        in_=w_down[l, 1024 * i:1024 * (i + 1), :].rearrange("(k p) n -> p k n", p=128))),
                           bwd[i], writes=[bwd[i]])
                g2 = sbt(es, "g2", [128, D], F32)
                bG = Buf("p3consts")
                S_.dma(SP, lambda e: e.dma_start(out=g2[:], in_=mlp_norm[l:l + 1, :].partition_broadcast(128)),
                       bG, pwrites=[bG])
                yT = [sbt(es, "yT%d" % i, [128, 8, TC], BF16) for i in range(2)]
                byT = [Buf("yT%d" % i) for i in range(2)]
                xt = [sbt(es, "x3_%d" % i, [128, D], F32) for i in range(4)]
                bxt = [Buf("x3_%d" % i) for i in range(4)]
                st8 = sbt(es, "st83", [128, 8], F32)
                bss = [Buf("ss3_%d" % i) for i in range(2)]
                brs = Buf("rs3")
                xs = [sbt(es, "xs3_%d" % i, [128, D], BF16) for i in range(2)]
                bxs = [Buf("xs3_%d" % i) for i in range(2)]
                hnT = [sbt(es, "hn2T%d" % i, [128, 8, TC], BF16) for i in range(2)]
                bhn = [Buf("hn2T%d" % i) for i in range(2)]
                hT = sbt(es, "hT", [128, 32, TC], BF16)
                bhT = Buf("hT")
                rl = [sbt(es, "rl%d" % i, [128, 2 * TC], BF16) for i in range(2)]
                brl = [Buf("rl%d" % i) for i in range(2)]
                NC3 = S // TC
                NTT = TC // 128

                def loadY(c):
                    cb = c % 2
                    S_.dma(SP, (lambda e: e.dma_start(out=yT[cb][:], in_=YT.rearrange("(k p) s -> p k s", p=128)[:, :, c * TC:(c + 1) * TC])),
                           byT[cb], reads=[bYT], writes=[byT[cb]])

                def stageA1(c):
                    cb = c % 2
                    for tt in range(NTT):
                        k4 = (c * NTT + tt) % 4
                        t0 = c * TC + tt * 128
                        S_.dma(SP, (lambda e, k4=k4, t0=t0: e.dma_start(out=xt[k4][:], in_=x_src[t0:t0 + 128, :])),
                               bxt[k4], reads=[bx_src], writes=[bxt[k4]])
                    for tt in range(NTT):
                        k4 = (c * NTT + tt) % 4
                        k = tt % 2
                        t0 = c * TC + tt * 128
                        ti = t0 // 128
                        for half in range(2):
                            for g, kcs in ((0, (0, 1)), (1, (2, 3, 4)), (2, (5, 6, 7))):
                                pb = (half * 3 + g) % 2

                                def f(e, half=half, tt=tt, kcs=kcs, pb=pb):
                                    ins = None
                                    for kc in kcs:
                                        ins = e.matmul(pbank[pb][:], lhsT=yT[cb][:, kc, tt * 128:(tt + 1) * 128],
                                                       rhs=wo[:, kc, half * 512:(half + 1) * 512],
                                                       start=(kc == kcs[0]), stop=(kc == kcs[-1]))
                                    return ins
                                S_.op(PE, f, reads=[byT[cb]] + bwo, writes=[bP[pb]])
                                S_.op(DVE, (lambda e, half=half, k4=k4, pb=pb, g=g, ti=ti: e.scalar_tensor_tensor(
                                    out=xt[k4][:, half * 512:(half + 1) * 512], in0=pbank[pb][:],
                                    scalar=RG[:, ti, g:g + 1], in1=xt[k4][:, half * 512:(half + 1) * 512],
                                    op0=ALU.mult, op1=ALU.add)),
                                      reads=[bP[pb], bRG], writes=[bxt[k4]])
                        if debug and XM is not None:
                            S_.dma(SP, (lambda e, k4=k4, t0=t0: e.dma_start(out=XM[t0:t0 + 128, :], in_=xt[k4][:])),
                                   bxt[k4], store=True, reads=[bxt[k4]], pwrites=[bXM])
                    for tt in range(NTT):
                        k4 = (c * NTT + tt) % 4
                        S_.op(ACT, (lambda e, tt=tt, k4=k4: e.activation(out=xs[tt][:], in_=xt[k4][:], func=AF.Square)),
                              reads=[bxt[k4]], writes=[bxs[tt]])
                    for tt in range(NTT):
                        S_.op(DVE, (lambda e, tt=tt: e.tensor_reduce(out=st8[:, tt:tt + 1], in_=xs[tt][:], axis=AX.X,
                                                                     op=ALU.add)),
                              reads=[bxs[tt]], writes=[bss[tt]])
                    S_.op(ACT, lambda e: e.activation(out=st8[:, 4:4 + NTT], in_=st8[:, 0:NTT], func=AF.Ln, bias=epsb[:],
                                                      scale=1.0 / D),
                          reads=bss + [bC], writes=[brs])
                    S_.op(ACT, lambda e: e.activation(out=st8[:, 4:4 + NTT], in_=st8[:, 4:4 + NTT], func=AF.Exp,
                                                      scale=-0.5), writes=[brs])
                    for tt in range(NTT):
                        k4 = (c * NTT + tt) % 4
                        S_.op(DVE, (lambda e, tt=tt, k4=k4: e.scalar_tensor_tensor(out=xs[tt][:], in0=xt[k4][:],
                                                                                   scalar=st8[:, 4 + tt:5 + tt],
                                                                                   in1=g2[:], op0=ALU.mult,
                                                                                   op1=ALU.mult)),
                              reads=[bxt[k4], brs, bG], writes=[bxs[tt]])

                def stageA2(c):
                    cb = c % 2
                    for tt in range(NTT):
                        k = tt % 2
                        pbf = pbank[2][:].bitcast(BF16)

                        def tr(e, k=k, pbf=pbf):
                            ins = None
                            for dc in range(8):
                                ins = e.transpose(pbf[:, dc * 128:(dc + 1) * 128], xs[k][:, dc * 128:(dc + 1) * 128],
                                                  ident[:])
                            return ins
                        S_.op(PE, tr, reads=[bxs[k], bC], writes=[bP[2]])
                        S_.op(ACT, (lambda e, tt=tt, pbf=pbf: e.copy(
                            out=hnT[cb][:, :, tt * 128:(tt + 1) * 128],
                            in_=pbf.rearrange("p (c t) -> p c t", c=8))),
                              reads=[bP[2]], pwrites=[bhn[cb]])

                def stageBup(c):
                    cb = c % 2
                    for f2 in range(16):
                        pu = 2 + (f2 % 2)
                        ri = f2 % 2

                        def f(e, f2=f2, pu=pu):
                            ins = None
                            for s in range(2):
                                fc = f2 * 2 + s
                                for dc in range(8):
                                    ins = e.matmul(pbank[pu][:, s * TC:(s + 1) * TC],
                                                   lhsT=wu[:, dc, fc * 128:(fc + 1) * 128], rhs=hnT[cb][:, dc, :],
                                                   start=(dc == 0), stop=(dc == 7))
                            return ins
                        S_.op(PE, f, reads=bwu + [bhn[cb]], writes=[bP[pu]])
                        S_.op(ACT, (lambda e, pu=pu, ri=ri: e.activation(out=rl[ri][:], in_=pbank[pu][:],
                                                                         func=AF.Relu)),
                              reads=[bP[pu]], writes=[brl[ri]])
                        eng = POOL if f2 % 2 else DVE
                        S_.op(eng, (lambda e, f2=f2, ri=ri: e.tensor_tensor(
                            out=hT[:, 2 * f2:2 * f2 + 2, :], in0=rl[ri][:].rearrange("p (s t) -> p s t", s=2),
                            in1=rl[ri][:].rearrange("p (s t) -> p s t", s=2), op=ALU.mult)),
                              reads=[brl[ri]], pwrites=[bhT])

                def stageBdown(c):
                    def f(e):
                        ins = None
                        for fc in range(32):
                            for tt in range(NTT):
                                for half in range(2):
                                    ins = e.matmul(pbank[4 + tt * 2 + half][:],
                                                   lhsT=hT[:, fc, tt * 128:(tt + 1) * 128],
                                                   rhs=wd[:, fc, half * 512:(half + 1) * 512],
                                                   start=(fc == 0), stop=(fc == 31))
                        return ins
                    S_.op(PE, f, reads=[bhT] + bwd, writes=[bP[4 + i] for i in range(2 * NTT)])
                    for tt in range(NTT):
                        k4 = (c * NTT + tt) % 4
                        t0 = c * TC + tt * 128
                        for half in range(2):
                            S_.op(DVE, (lambda e, tt=tt, half=half, k4=k4: e.tensor_tensor(
                                out=xt[k4][:, half * 512:(half + 1) * 512], in0=pbank[4 + tt * 2 + half][:],
                                in1=xt[k4][:, half * 512:(half + 1) * 512], op=ALU.add)),
                                  reads=[bP[4 + tt * 2 + half]], writes=[bxt[k4]])
                        S_.dma(SP, (lambda e, k4=k4, t0=t0: e.dma_start(out=x_dst[t0:t0 + 128, :], in_=xt[k4][:])),
                               bxt[k4], store=True, reads=[bxt[k4]], pwrites=[bx_dst])

                loadY(0)
                loadY(1)
                stageA1(0)
                stageA2(0)
                for c in range(NC3):
                    if c + 1 < NC3:
                        stageA1(c + 1)
                    if c + 2 < NC3:
                        loadY(c + 2)
                    stageBup(c)
                    if c + 1 < NC3:
                        stageA2(c + 1)
                    stageBdown(c)
            S_.barrier()

        bxin = Buf("x_in")
        for l in range(NL):
            x_src, bsrc = (x_in, bxin) if l == 0 else (X1, bX1)
            x_dst, bdst = (out, bOUT) if l == NL - 1 else (X1, bX1)
            phase1(l, x_src, bsrc)
            with ExitStack() as wes:
                w3a = load_w3a(l, wes)
                phase2(l)
                phase3(l, x_src, bsrc, x_dst, bdst, w3a)
        S_.emit(final_waits=_compress(bOUT.writers))
        S_.close()
    return nc, consts_np


def _prep_weights(inp, NL):
    f = lambda a: np.ascontiguousarray(np.asarray(a, dtype=np.float32))
    qg = f(inp["q_gain"])
    kg = f(inp["k_gain"])
    og = f(inp["out_gain"])
    m = {
        "attn_norm": f(inp["attn_norm"])[:NL],
        "w_in": f(inp["w_in"])[:NL],
        "b_forget": f(inp["b_forget"])[:NL].reshape(NL, N_FOX, 1),
        "qg_col": np.ascontiguousarray(qg[:NL].reshape(NL, 8, 2, 64).transpose(0, 2, 3, 1).reshape(NL, 128, 8)),
        "kg_col": np.ascontiguousarray(kg[:NL].reshape(NL, 8, 2, 64).transpose(0, 2, 3, 1).reshape(NL, 128, 8)),
        "og_col": np.ascontiguousarray(og[:NL].reshape(NL, 16, 64).transpose(0, 2, 1)),
        "w_out": f(inp["w_out"])[:NL],
        "mlp_norm": f(inp["mlp_norm"])[:NL],
        "w_up": f(inp["w_up"])[:NL],
        "w_down": f(inp["w_down"])[:NL],
    }
    return m


_CACHE = {}


def kernel(**inputs):
    x = np.asarray(inputs["x"], dtype=np.float32)
    B, S, _ = x.shape
    NL = 2
    key = (S, NL)
    if key not in _CACHE:
        _CACHE[key] = build(S, NL)
    nc, consts = _CACHE[key]
    wm = _prep_weights(inputs, NL)
    for k, v in consts.items():
        wm["c_" + k] = v
    in_maps = []
    for b in range(B):
        m = dict(wm)
        m["x"] = np.ascontiguousarray(x[b])
        in_maps.append(m)
    res = run_bass_kernel_spmd(nc, in_maps, core_ids=list(range(B)))
    return np.stack([np.asarray(r["out"], dtype=np.float32) for r in res.results], axis=0)
```
